# Optimizing a Trainium2 kernel written in Bass

```python
import math
import jax, jax.numpy as jnp
from jax import lax
import numpy as np

D_MODEL = 2048
BATCH = 2
SEQ = 16384
DEPTH = 2

CHUNK = 64
LEFT_CHUNKS = 8
BAND = (LEFT_CHUNKS + 1) * CHUNK
A_HEADS = 32
A_HEAD_DIM = D_MODEL // A_HEADS
REL_CLIP = 2 * CHUNK
B_HEADS = 16
B_HEAD_DIM = D_MODEL // B_HEADS
Q_BLOCK = 128
N_A_LAYERS = DEPTH // 2
N_EXPERTS = 16
N_GROUPS = 4
EXPERTS_PER_GROUP = N_EXPERTS // N_GROUPS
TOP_K = 2
D_FF = 2048
EXPERT_BLOCK = 256
ALPHA = (2.0 * DEPTH) ** 0.25
BETA = (8.0 * DEPTH) ** -0.25
LN_EPS = 1e-5
NEG = -1e30

kernel_name = "yoco_chunk_relpos_fox_grouped_moe_deepnorm"


def layer_norm(x, gain, bias):
    xf = x.astype(jnp.float32)
    mu = jnp.mean(xf, axis=-1, keepdims=True)
    var = jnp.mean(jnp.square(xf - mu), axis=-1, keepdims=True)
    y = (xf - mu) * lax.rsqrt(var + LN_EPS) * gain.astype(jnp.float32) + bias.astype(jnp.float32)
    return y.astype(x.dtype)


def chunk_relpos_attention(x, w_qkv, rel_bias, w_o):
    b, s, d = x.shape
    nc = s // CHUNK
    pad = LEFT_CHUNKS * CHUNK
    q, k, v = jnp.split(x @ w_qkv, 3, axis=-1)
    q = q.reshape(b, nc, CHUNK, A_HEADS, A_HEAD_DIM).transpose(1, 0, 2, 3, 4)
    k = jnp.pad(k.reshape(b, s, A_HEADS, A_HEAD_DIM), ((0, 0), (pad, 0), (0, 0), (0, 0)))
    v = jnp.pad(v.reshape(b, s, A_HEADS, A_HEAD_DIM), ((0, 0), (pad, 0), (0, 0), (0, 0)))
    rel = jnp.arange(CHUNK)[:, None] - jnp.arange(BAND)[None, :] + pad
    bias = rel_bias[:, jnp.clip(rel, -REL_CLIP, REL_CLIP) + REL_CLIP].astype(jnp.float32)
    scale = 1.0 / math.sqrt(A_HEAD_DIM)

    def one_chunk(args):
        qc, c = args
        kc = lax.dynamic_slice_in_dim(k, c * CHUNK, BAND, axis=1)
        vc = lax.dynamic_slice_in_dim(v, c * CHUNK, BAND, axis=1)
        sc = jnp.einsum('bqhd,bkhd->bhqk', qc, kc).astype(jnp.float32) * scale + bias[None]
        key_pos = c * CHUNK - pad + jnp.arange(BAND)
        sc = jnp.where((key_pos >= 0)[None, None, None, :], sc, NEG)
        p = jax.nn.softmax(sc, axis=-1)
        return jnp.einsum('bhqk,bkhd->bqhd', p.astype(vc.dtype), vc)

    out = lax.map(one_chunk, (q, jnp.arange(nc)))
    out = out.transpose(1, 0, 2, 3, 4).reshape(b, s, d)
    return out @ w_o


def shared_kv_side(x, kv_w, fg_w, fg_b):
    b, s, d = x.shape
    k, v = jnp.split(x @ kv_w, 2, axis=-1)
    k = k.reshape(b, s, B_HEADS, B_HEAD_DIM)
    v = v.reshape(b, s, B_HEADS, B_HEAD_DIM)
    log_f = jax.nn.log_sigmoid((x @ fg_w).astype(jnp.float32) + fg_b.astype(jnp.float32))
    f_cum = jnp.cumsum(log_f, axis=1)
    return k, v, f_cum


def forgetting_attention(x, w_q, w_o, k, v, f_cum):
    b, s, d = x.shape
    nq = s // Q_BLOCK
    q = (x @ w_q).reshape(b, nq, Q_BLOCK, B_HEADS, B_HEAD_DIM).transpose(1, 0, 2, 3, 4)
    fq = f_cum.reshape(b, nq, Q_BLOCK, B_HEADS).transpose(1, 0, 3, 2)
    fk = f_cum.transpose(0, 2, 1)
    key_pos = jnp.arange(s)
    scale = 1.0 / math.sqrt(B_HEAD_DIM)

    def one_block(args):
        qb, fqb, i = args
        sc = jnp.einsum('bqhd,bkhd->bhqk', qb, k).astype(jnp.float32) * scale
        sc = sc + fqb[..., None] - fk[:, :, None, :]
        q_pos = i * Q_BLOCK + jnp.arange(Q_BLOCK)
        sc = jnp.where((key_pos[None, :] <= q_pos[:, None])[None, None], sc, NEG)
        p = jax.nn.softmax(sc, axis=-1)
        return jnp.einsum('bhqk,bkhd->bqhd', p.astype(v.dtype), v)

    out = lax.map(one_block, (q, fq, jnp.arange(nq)))
    out = out.transpose(1, 0, 2, 3, 4).reshape(b, s, d)
    return out @ w_o


def grouped_top2_route(xt, router_w, router_b):
    logits = (xt @ router_w).astype(jnp.float32) + router_b.astype(jnp.float32)
    probs = jax.nn.softmax(logits, axis=-1)
    pg = probs.reshape(-1, N_GROUPS, EXPERTS_PER_GROUP)
    group_score = jnp.sum(lax.top_k(pg, TOP_K)[0], axis=-1)
    g = jnp.argmax(group_score, axis=-1)
    p_in = jnp.take_along_axis(pg, g[:, None, None], axis=1)[:, 0]
    vals, loc = lax.top_k(p_in, TOP_K)
    experts = (g[:, None] * EXPERTS_PER_GROUP + loc).astype(jnp.int32)
    gates = vals / jnp.sum(vals, axis=-1, keepdims=True)
    return experts, gates


def moe_ffn(xt, experts, gates, w_gate, w_up, w_down):
    n, d = xt.shape
    a = n * TOP_K
    e_flat = experts.reshape(-1)
    tok_flat = jnp.repeat(jnp.arange(n, dtype=jnp.int32), TOP_K)
    g_flat = gates.reshape(-1)
    counts = jnp.bincount(e_flat, length=N_EXPERTS)
    starts = jnp.cumsum(counts) - counts
    padded = (counts + EXPERT_BLOCK - 1) // EXPERT_BLOCK * EXPERT_BLOCK
    pend = jnp.cumsum(padded)
    pstart = pend - padded
    order = jnp.argsort(e_flat)
    e_sorted = e_flat[order]
    dest = pstart[e_sorted] + jnp.arange(a) - starts[e_sorted]
    n_blocks = -(-a // EXPERT_BLOCK) + N_EXPERTS
    p = n_blocks * EXPERT_BLOCK
    slot_tok = jnp.full((p,), n, jnp.int32).at[dest].set(tok_flat[order])
    slot_gate = jnp.zeros((p,), jnp.float32).at[dest].set(g_flat[order])
    block_expert = jnp.minimum(
        jnp.searchsorted(pend, jnp.arange(n_blocks) * EXPERT_BLOCK, side='right'), N_EXPERTS - 1)
    x_pad = jnp.concatenate([xt, jnp.zeros((1, d), xt.dtype)], axis=0)
    xb = x_pad[slot_tok].reshape(n_blocks, EXPERT_BLOCK, d)

    def run_block(args):
        xblk, e = args
        hid = jax.nn.silu(xblk @ w_gate[e]) * (xblk @ w_up[e])
        return hid @ w_down[e]

    yb = lax.map(run_block, (xb, block_expert)).reshape(p, d)
    y = jnp.zeros((n + 1, d), yb.dtype).at[slot_tok].add(yb * slot_gate[:, None].astype(yb.dtype))
    return y[:n]


def setup_inputs(seed: int = 0) -> dict:
    key = jax.random.key(seed)
    ks = jax.random.split(key, 20)
    n_a = N_A_LAYERS
    n_b = DEPTH - N_A_LAYERS
    d = D_MODEL
    s = d ** -0.5
    nrm = jax.random.normal
    x = nrm(ks[0], (BATCH, SEQ, d), jnp.float32)
    a_w_qkv = jnp.concatenate([nrm(ks[1], (n_a, d, 2 * d)) * s,
                               nrm(ks[2], (n_a, d, d)) * (s * BETA)], axis=-1)
    a_rel_bias = 0.2 * nrm(ks[3], (n_a, A_HEADS, 2 * REL_CLIP + 1))
    a_w_o = nrm(ks[4], (n_a, d, d)) * (s * BETA)
    kv_w = jnp.concatenate([nrm(ks[5], (d, d)) * s, nrm(ks[6], (d, d)) * (s * BETA)], axis=-1)
    fg_w = nrm(ks[7], (d, B_HEADS)) * s
    fg_b = 2.0 + 2.0 * jax.random.uniform(ks[8], (B_HEADS,))
    b_w_q = nrm(ks[9], (n_b, d, d)) * s
    b_w_o = nrm(ks[10], (n_b, d, d)) * (s * BETA)
    router_w = nrm(ks[11], (d, N_EXPERTS)) * s
    router_b = 0.01 * nrm(ks[12], (N_EXPERTS,))
    moe_w_gate = nrm(ks[13], (DEPTH, N_EXPERTS, d, D_FF)) * s
    moe_w_up = nrm(ks[14], (DEPTH, N_EXPERTS, d, D_FF)) * (s * BETA)
    moe_w_down = nrm(ks[15], (DEPTH, N_EXPERTS, D_FF, d)) * (D_FF ** -0.5 * BETA)
    ln_gain = 1.0 + 0.02 * nrm(ks[16], (DEPTH, 2, d))
    ln_bias = 0.02 * nrm(ks[17], (DEPTH, 2, d))
    return {"x": x, "a_w_qkv": a_w_qkv, "a_rel_bias": a_rel_bias, "a_w_o": a_w_o,
            "kv_w": kv_w, "fg_w": fg_w, "fg_b": fg_b, "b_w_q": b_w_q, "b_w_o": b_w_o,
            "router_w": router_w, "router_b": router_b, "moe_w_gate": moe_w_gate,
            "moe_w_up": moe_w_up, "moe_w_down": moe_w_down, "ln_gain": ln_gain,
            "ln_bias": ln_bias}


def reference(x, a_w_qkv, a_rel_bias, a_w_o, kv_w, fg_w, fg_b, b_w_q, b_w_o,
              router_w, router_b, moe_w_gate, moe_w_up, moe_w_down, ln_gain, ln_bias):
    b, s, d = x.shape
    k_sh = v_sh = f_sh = None
    for layer in range(DEPTH):
        if layer < N_A_LAYERS:
            h = chunk_relpos_attention(x, a_w_qkv[layer], a_rel_bias[layer], a_w_o[layer])
        else:
            lb = layer - N_A_LAYERS
            h = forgetting_attention(x, b_w_q[lb], b_w_o[lb], k_sh, v_sh, f_sh)
        x = layer_norm(ALPHA * x + h, ln_gain[layer, 0], ln_bias[layer, 0])
        xt = x.reshape(b * s, d)
        experts, gates = grouped_top2_route(xt, router_w, router_b)
        y = moe_ffn(xt, experts, gates, moe_w_gate[layer], moe_w_up[layer], moe_w_down[layer])
        x = layer_norm(ALPHA * x + y.reshape(b, s, d), ln_gain[layer, 1], ln_bias[layer, 1])
        if layer == N_A_LAYERS - 1:
            k_sh, v_sh, f_sh = shared_kv_side(x, kv_w, fg_w, fg_b)
    return x
```

```python
import math
import numpy as np
import concourse.bass as bass
import concourse.mybir as mybir
from concourse.bass_utils import run_bass_kernel_spmd
from contextlib import ExitStack

F32 = mybir.dt.float32
BF16 = mybir.dt.bfloat16
I32 = mybir.dt.int32
ALU = mybir.AluOpType
AF = mybir.ActivationFunctionType
AX = mybir.AxisListType

D = 2048
KC = 16
NT = 4096
HALO = 512
NTH = NT + HALO
SEQ = 16384
NCORES = 8
CAP = 768
NSLOT = 16 * CAP
ALPHA = (2.0 * 2) ** 0.25
LN_EPS = 1e-5
BIGSLOT = 1.0e6

ENGS = ('pe', 'act', 'dve', 'pool', 'sp')
ROT = 30000


class Op:
    __slots__ = ('eng', 'fn', 'deps', 'signal', 'ms', 'dma_key', 'dma_val', 'is_dma')


class Prog:
    def __init__(self, nc):
        self.nc = nc
        self.ops = {e: [] for e in ENGS}
        self.last_w = {}
        self.readers = {}
        self.dma_cnt = {}
        self.stack = ExitStack()
        self.n_names = 0
        self.banks = None
        self.bank_i = 0
        self.ev_i = 0

    def init_arena(self, nbytes=200 * 1024):
        self.arena = self.stack.enter_context(self.nc.sbuf_tensor("arena", [128, nbytes // 4], F32))
        self.arena_top = 0
        self.arena_size = nbytes

    def sb(self, shape, dt, name=None):
        esz = {F32: 4, I32: 4, BF16: 2}[dt]
        nel = 1
        for d in shape[1:]:
            nel *= d
        nbytes = (nel * esz + 63) // 64 * 64
        off = self.arena_top
        assert off + nbytes <= self.arena_size, (name, off, nbytes)
        self.arena_top = off + nbytes
        v = self.arena[0:shape[0], off // 4:(off + nel * esz + 3) // 4]
        if dt != F32:
            v = v.bitcast(dt)
        if esz == 2 and nel % 2 == 1:
            v = v[:, 0:nel]
        if len(shape) == 3:
            v = v.rearrange("q (a b) -> q a b", a=shape[1])
        elif len(shape) == 4:
            v = v.rearrange("q (a b c) -> q a b c", a=shape[1], b=shape[2])
        return v

    def mark(self):
        return self.arena_top

    def release(self, m):
        self.barrier()
        self.arena_top = m

    def barrier(self):
        lasts = {}
        for e in ENGS:
            for op in reversed(self.ops[e]):
                if op.fn is not None and not op.is_dma:
                    lasts[e] = op
                    break
        dmas = [('dma', k, 16 * c) for k, c in self.dma_cnt.items()]
        for e in ENGS:
            op = Op()
            op.eng = e
            op.fn = None
            op.signal = False
            op.ms = None
            op.is_dma = False
            op.dma_key = None
            op.dma_val = None
            op.deps = list(dmas)
            for e2, l in lasts.items():
                if e2 != e:
                    l.signal = True
                    op.deps.append(('op', l))
            self.ops[e].append(op)
        self.last_w = {}
        self.readers = {}

    def make_banks(self):
        self.banks = []
        for i in range(8):
            self.banks.append(self.stack.enter_context(self.nc.psum_tensor(f"bank{i}", [128, 512], F32)))

    def bank(self):
        i = self.bank_i
        self.bank_i = (i + 1) % 8
        return ('ps', i), self.banks[i]

    def _add(self, eng, fn, reads, writes, is_dma=False, dma_key=None):
        op = Op()
        op.eng = eng
        op.fn = fn
        op.signal = False
        op.ms = None
        op.is_dma = is_dma
        op.dma_key = dma_key
        op.dma_val = None
        deps = {}
        for k in reads:
            w = self.last_w.get(k)
            if w is not None:
                deps[id(w)] = (w, 'RAW')
        for k in writes:
            w = self.last_w.get(k)
            if w is not None and id(w) not in deps:
                deps[id(w)] = (w, 'WAW')
            for r in self.readers.get(k, ()):
                if id(r) not in deps:
                    deps[id(r)] = (r, 'WAR')
        waits = []
        for d, kind in deps.values():
            if d.is_dma:
                waits.append(('dma', d.dma_key, d.dma_val))
            else:
                if d.eng == eng and not is_dma:
                    if eng == 'pe':
                        continue
                    if kind != 'RAW':
                        continue
                d.signal = True
                waits.append(('op', d))
        op.deps = waits
        if is_dma:
            c = self.dma_cnt.get(dma_key, 0) + 1
            self.dma_cnt[dma_key] = c
            op.dma_val = 16 * c
        for k in reads:
            self.readers.setdefault(k, []).append(op)
        for k in writes:
            self.last_w[k] = op
            self.readers[k] = []
        self.ops[eng].append(op)
        return op

    def I(self, eng, method, reads, writes, *a, **kw):
        return self._add(eng, lambda e: getattr(e, method)(*a, **kw), tuple(reads), tuple(writes))

    def mm(self, out, lhsT, rhs, start, stop, reads, writes, **kw):
        return self._add('pe', lambda e: e.matmul(out, lhsT=lhsT, rhs=rhs, start=start, stop=stop, **kw),
                         tuple(reads), tuple(writes))

    def tr(self, out, in_, ident, reads, writes):
        return self._add('pe', lambda e: e.transpose(out=out, in_=in_, identity=ident), tuple(reads), tuple(writes))

    def evac(self, out, in_, reads, writes, eng=None):
        if eng is None:
            eng = ('act', 'dve')[self.ev_i % 2]
            self.ev_i += 1
        if eng == 'act':
            return self.I('act', 'copy', reads, writes, out=out, in_=in_)
        return self.I(eng, 'tensor_copy', reads, writes, out=out, in_=in_)

    def dma(self, queue, out, in_, reads=(), writes=(), key=None, **kw):
        if key is None:
            key = ('dmak',) + tuple(writes)
        return self._add(queue, lambda e: e.dma_start(out=out, in_=in_, **kw),
                         tuple(reads), tuple(writes), is_dma=True, dma_key=key)

    def dma_fn(self, queue, fn, reads=(), writes=(), key=None):
        if key is None:
            key = ('dmak',) + tuple(writes)
        return self._add(queue, fn, tuple(reads), tuple(writes), is_dma=True, dma_key=key)

    def _bound_reg(self, e, bound):
        regs = self.__dict__.setdefault('bound_regs', {})
        if bound not in regs:
            r = e.alloc_register(f"bnd{bound}")
            e.reg_mov(r, bound)
            regs[bound] = r
        return regs[bound]

    def gather(self, out, table, idx_ap, bound, reads, writes, key=None):
        return self.dma_fn('pool', lambda e: e.indirect_dma_start(
            out=out, out_offset=None, in_=table,
            in_offset=bass.IndirectOffsetOnAxis(ap=idx_ap, axis=0),
            bounds_check=self._bound_reg(e, bound), oob_is_err=False), reads, writes, key)

    def scatter(self, table, idx_ap, src, bound, reads, writes, key=None):
        return self.dma_fn('pool', lambda e: e.indirect_dma_start(
            out=table, out_offset=bass.IndirectOffsetOnAxis(ap=idx_ap, axis=0),
            in_=src, in_offset=None, bounds_check=self._bound_reg(e, bound), oob_is_err=False), reads, writes, key)

    def fence(self, eng, keys):
        return self._add(eng, None, tuple(keys), ())

    def emit(self):
        nc = self.nc
        st = self.stack
        eng_sems = {e: [] for e in ENGS}
        for e in ENGS:
            n = 0
            for op in self.ops[e]:
                if op.is_dma or not op.signal:
                    continue
                si, v = divmod(n, ROT)
                n += 1
                while len(eng_sems[e]) <= si:
                    eng_sems[e].append(st.enter_context(nc.semaphore(f"s_{e}_{len(eng_sems[e])}")))
                op.ms = (eng_sems[e][si], v + 1)
        dma_sems = {}
        for i, k in enumerate(self.dma_cnt):
            assert 16 * self.dma_cnt[k] < 60000, (k, self.dma_cnt[k])
            dma_sems[k] = st.enter_context(nc.semaphore(f"s_dma_{i}"))
        self.n_sems = sum(len(v) for v in eng_sems.values()) + len(dma_sems)

        def run(e, eng):
            waited = {}
            for op in self.ops[e]:
                for w in op.deps:
                    if w[0] == 'dma':
                        sem, val = dma_sems[w[1]], w[2]
                    else:
                        sem, val = w[1].ms
                    if waited.get(id(sem), 0) >= val:
                        continue
                    waited[id(sem)] = val
                    eng.wait_ge(sem, val)
                if op.fn is None:
                    continue
                inst = op.fn(eng)
                if op.is_dma:
                    inst.then_inc(dma_sems[op.dma_key], 16)
                elif op.ms is not None:
                    inst.then_inc(op.ms[0], 1)

        with nc.Block() as block:
            @block.tensor
            def _(eng):
                run('pe', eng)

            @block.scalar
            def _(eng):
                run('act', eng)

            @block.vector
            def _(eng):
                run('dve', eng)

            @block.gpsimd
            def _(eng):
                run('pool', eng)

            @block.sync
            def _(eng):
                run('sp', eng)
        st.close()


class Consts:
    pass


def load_consts(p, cd):
    c = Consts()
    identf = p.sb([128, 128], F32, 'identf')
    p.dma('sp', identf[:], cd['ident'], writes=['identf'])
    identb = p.sb([128, 128], BF16, 'identb')
    p.I('dve', 'tensor_copy', ['identf'], ['identb'], out=identb[:], in_=identf[:])
    c.identf, c.identb = identf, identb
    return c


def stage_transpose(p, c, x_rows, ntok, xT_out, xT_key, tag):
    nblk = ntok // 128
    xb = [p.sb([128, D], BF16, f'{tag}_xb{i}') for i in range(2)]
    stg = [p.sb([128, KC, 512], BF16, f'{tag}_stg{i}') for i in range(2)]
    xT_v = xT_out.rearrange("(kc q) t -> q kc t", q=128)
    for tb in range(nblk):
        b = xb[tb % 2]
        bk = (tag, 'xb', tb % 2)
        p.dma('pool', b[:], x_rows[tb * 128:(tb + 1) * 128, :], writes=[bk])
        tile_i = tb // 4
        s = stg[tile_i % 2]
        sk = (tag, 'stg', tile_i % 2)
        for half in range(2):
            pk, pb = p.bank()
            pbv = pb[:].bitcast(BF16)
            for j in range(8):
                kc = half * 8 + j
                p.tr(pbv[:, j * 128:(j + 1) * 128], b[:, kc * 128:(kc + 1) * 128], c.identb[:],
                     [bk, 'identb'], [pk])
            p.evac(s[:, half * 8:(half + 1) * 8, (tb % 4) * 128:(tb % 4 + 1) * 128],
                   pbv.rearrange("q (k t) -> q k t", k=8), [pk], [sk])
        if tb % 4 == 3:
            p.dma('sp', xT_v[:, :, tile_i * 512:(tile_i + 1) * 512], s[:], reads=[sk], writes=[xT_key])


def load_wres(p, wres, wkey, w_ap, F):
    wv = w_ap.rearrange("(kc q) f -> q kc f", q=128)
    for c0 in range(0, F, 512):
        c1 = min(F, c0 + 512)
        p.dma('pool', wres[:, :, c0:c1], wv[:, :, c0:c1], writes=[wkey], key=('wld', wkey))


def stage_proj(p, xT_in, xT_key, ntok, wres, wkey, F, mode, out, out_key, tag, bufs):
    xts, osts = bufs
    xT_v = xT_in.rearrange("(kc q) t -> q kc t", q=128)
    ntile = ntok // 512
    for tt in range(ntile):
        xt = xts[tt % 2]
        xk = (tag, 'xt', tt % 2)
        p.dma('sp', xt[:], xT_v[:, :, tt * 512:(tt + 1) * 512], reads=[xT_key], writes=[xk])
        if mode == 'fm':
            ost = osts[tt % 2]
            ok = (tag, 'ost', tt % 2)
            nfc = F // 128
            for fc in range(nfc):
                pk, pb = p.bank()
                for kc in range(KC):
                    p.mm(pb[:], wres[:, kc, fc * 128:(fc + 1) * 128], xt[:, kc, :], kc == 0, kc == KC - 1,
                         [wkey, xk], [pk])
                p.evac(ost[:, fc, :], pb[:], [pk], [ok])
            ov = out.rearrange("(fc q) t -> q fc t", q=128)
            p.dma('sp', ov[:, :, tt * 512:(tt + 1) * 512], ost[:, 0:nfc, :], reads=[ok], writes=[out_key])
        else:
            for tb in range(4):
                ost = osts[(tt * 4 + tb) % 2]
                ok = (tag, 'ost', (tt * 4 + tb) % 2)
                for c0 in range(0, F, 512):
                    c1 = min(F, c0 + 512)
                    pk, pb = p.bank()
                    for kc in range(KC):
                        p.mm(pb[:, 0:c1 - c0], xt[:, kc, tb * 128:(tb + 1) * 128], wres[:, kc, c0:c1],
                             kc == 0, kc == KC - 1, [wkey, xk], [pk])
                    p.evac(ost[:, c0:c1], pb[:, 0:c1 - c0], [pk], [ok])
                r0 = tt * 512 + tb * 128
                p.dma('sp', out[r0:r0 + 128, 0:F], ost[:, 0:F], reads=[ok], writes=[out_key])


def stage_attnA(p, c, QT, KT, V, kvalid_d, biasT_d, cvec_d, aoT, tag='aa'):
    etab = p.sb([128, 32, 256], BF16, 'etab')
    btmp = [p.sb([128, 8, 256], F32, f'btmp{i}') for i in range(1)]
    for g in range(4):
        bt = btmp[0]
        bk = ('btmp', 0)
        p.dma('sp', bt[:], biasT_d[:, g * 8:(g + 1) * 8, :], writes=[bk])
        p.I('act', 'activation', [bk], ['etab'], out=etab[:, g * 8:(g + 1) * 8, :], in_=bt[:], func=AF.Exp)
    p.I('dve', 'memset', [], ['etab'], etab[64:128, :, 128:192], 0.0)
    cb = p.sb([128, 32], F32, 'cbias')
    p.dma('sp', cb[:], cvec_d.partition_broadcast(128), writes=['cbias'])
    kval = p.sb([128, NTH // 128], F32, 'kval')
    p.dma('sp', kval[:], kvalid_d, writes=['kval'])
    nkb = NTH // 128
    vones = p.sb([128, nkb, 64], BF16, 'vones')
    p.I('dve', 'tensor_copy', ['kval'], ['vones'], out=vones[:],
        in_=kval[:].unsqueeze(2).to_broadcast([128, nkb, 64]))

    QT_v = QT.rearrange("(kc q) t -> q kc t", q=128)
    KT_v = KT.rearrange("(kc q) t -> q kc t", q=128)
    V_v = V.rearrange("(b q) f -> q b f", q=128)
    aoT_v = aoT.rearrange("(kc q) t -> q kc t", q=128)
    qts = [p.sb([128, KC, 512], BF16, f'aa_q{i}') for i in range(1)]
    kts = [p.sb([128, KC, 512], BF16, f'aa_k{i}') for i in range(3)]
    vts = [p.sb([128, 4, D], BF16, f'aa_v{i}') for i in range(3)]
    aos = [p.sb([128, KC, 512], BF16, f'aa_o{i}') for i in range(2)]
    pts = [p.sb([128, 640], BF16, f'aa_p{i}') for i in range(3)]
    rdn = [p.sb([128, 128], F32, f'aa_r{i}') for i in range(2)]

    def load_kv(j):
        p.dma('sp', kts[j % 3][:], KT_v[:, :, j * 512:(j + 1) * 512], reads=['KT'], writes=[('aa_k', j % 3)])
        p.dma('sp', vts[j % 3][:], V_v[:, j * 4:(j + 1) * 4, :], reads=['V'], writes=[('aa_v', j % 3)])

    load_kv(0)
    load_kv(1)
    ntile = NT // 512
    hp = 0
    for tt in range(ntile):
        if tt + 2 <= ntile:
            load_kv(tt + 2)
        qt = qts[0]
        qk = ('aa_q', 0)
        p.dma('sp', qt[:], QT_v[:, :, tt * 512:(tt + 1) * 512], reads=['QT'], writes=[qk])
        ao = aos[tt % 2]
        aok = ('aa_o', tt % 2)
        for qi in range(4):
            for pr in range(16):
                ok_, ob = p.bank()
                for hh in range(2):
                    h = pr * 2 + hh
                    pb0 = hh * 64
                    xk_, xb_ = p.bank()
                    yk_, yb_ = p.bank()
                    blks = []
                    for kb in range(5):
                        w = qi + kb
                        j = tt + (w // 4)
                        blk = w % 4
                        blks.append((j, blk))
                        dst = xb_[:, kb * 128:(kb + 1) * 128] if kb < 3 else yb_[:, (kb - 3) * 128:(kb - 2) * 128]
                        p.mm(dst, kts[j % 3][pb0:pb0 + 64, pr, blk * 128:(blk + 1) * 128],
                             qt[pb0:pb0 + 64, pr, qi * 128:(qi + 1) * 128], True, True,
                             [('aa_k', j % 3), qk], [xk_ if kb < 3 else yk_])
                    pt = pts[hp % 3]
                    ptk = ('aa_p', hp % 3)
                    hp += 1
                    p.I('act', 'activation', [xk_, 'cbias'], [ptk], out=pt[:, 0:384], in_=xb_[:, 0:384],
                        func=AF.Exp, bias=cb[:, h:h + 1], scale=0.125)
                    p.I('act', 'activation', [yk_], [ptk], out=pt[:, 384:640], in_=yb_[:, 0:256],
                        func=AF.Exp, scale=0.125)
                    p.I('pool', 'memset', [ptk], [ptk], pt[0:64, 64:128], 0.0)
                    p.I('dve', 'tensor_tensor', [ptk, 'etab'], [ptk], out=pt[:, 384:640], in0=pt[:, 384:640],
                        in1=etab[:, h, :], op=ALU.mult)
                    for kb in range(5):
                        j, blk = blks[kb]
                        p.mm(ob[pb0:pb0 + 64, 0:128], vts[j % 3][:, blk, h * 64:(h + 1) * 64],
                             pt[:, kb * 128:(kb + 1) * 128], kb == 0, kb == 4,
                             [('aa_v', j % 3), ptk], [ok_], tile_position=(0, pb0))
                    for kb in range(5):
                        j, blk = blks[kb]
                        p.mm(ob[pb0:pb0 + 64, 128:256], vones[:, j * 4 + blk, :],
                             pt[:, kb * 128:(kb + 1) * 128], kb == 0, kb == 4,
                             ['vones', ptk], [ok_], tile_position=(0, pb0))
                rd = rdn[pr % 2]
                rk = ('aa_r', pr % 2)
                p.I('dve', 'reciprocal', [ok_], [rk], out=rd[:], in_=ob[:, 128:256])
                p.I('dve', 'tensor_tensor', [ok_, rk], [aok], out=ao[:, pr, qi * 128:(qi + 1) * 128],
                    in0=ob[:, 0:128], in1=rd[:], op=ALU.mult)
        p.dma('sp', aoT_v[:, :, tt * 512:(tt + 1) * 512], ao[:], reads=[aok], writes=['aoT'])


def emit_ln(p, z, zk, G, B, tag, scr):
    stats, mv, rstd = scr
    sk = (tag, 'lnscr')
    for ch in range(4):
        p.I('dve', 'bn_stats', [zk], [sk], out=stats[:, ch, :], in_=z[:, ch * 512:(ch + 1) * 512])
    p.I('dve', 'bn_aggr', [sk], [sk], out=mv[:], in_=stats[:])
    p.I('act', 'activation', [sk], [sk], out=rstd[:], in_=mv[:, 1:2], func=AF.Sqrt, bias=LN_EPS, scale=1.0)
    p.I('dve', 'reciprocal', [sk], [sk], out=rstd[:], in_=rstd[:])
    p.I('dve', 'tensor_scalar', [zk, sk], [zk], out=z[:], in0=z[:], scalar1=mv[:, 0:1], scalar2=rstd[:],
        op0=ALU.subtract, op1=ALU.mult)
    p.I('pool', 'tensor_tensor', [zk, 'lnG'], [zk], out=z[:], in0=z[:], in1=G[:], op=ALU.mult)
    p.I('pool', 'tensor_tensor', [zk, 'lnB'], [zk], out=z[:], in0=z[:], in1=B[:], op=ALU.add)


def ln_scratch(p, tag):
    return (p.sb([128, 4, 6], F32, f'{tag}_st'), p.sb([128, 2], F32, f'{tag}_mv'), p.sb([128, 1], F32, f'{tag}_rs'))


def stage_tail1(p, c, aoT, aoT_key, xres, w_o, lng, lnb, rw, rb, cd, xm_f32, xm_bf, idx_tab, RT):
    wres = p.sb([128, KC, D], BF16, 't1_w')
    load_wres(p, wres, 't1_w', w_o, D)
    G = p.sb([128, D], F32, 't1_G')
    B = p.sb([128, D], F32, 't1_B')
    p.dma('sp', G[:], lng.partition_broadcast(128), writes=['lnG'])
    p.dma('sp', B[:], lnb.partition_broadcast(128), writes=['lnB'])
    wr = p.sb([128, KC, 16], F32, 't1_wr')
    p.dma('sp', wr[:], rw.rearrange("(kc q) e -> q kc e", q=128), writes=['wr'])
    rbb = p.sb([128, 16], F32, 't1_rb')
    p.dma('sp', rbb[:], rb.partition_broadcast(128), writes=['rbb'])
    aov = aoT.rearrange("(kc q) t -> q kc t", q=128)
    aos = [p.sb([128, KC, 512], BF16, f't1_ao{i}') for i in range(2)]
    xrs = [p.sb([128, D], F32, f't1_x{i}') for i in range(2)]
    zs = [p.sb([128, D], F32, f't1_z{i}') for i in range(2)]
    zb = [p.sb([128, D], BF16, f't1_zb{i}') for i in range(2)]
    xmT = [p.sb([128, KC, 128], F32, f't1_xmT{i}') for i in range(2)]
    scr = ln_scratch(p, 't1')
    lg = RT['lg']
    nblk = NT // 128
    for tb in range(nblk):
        tt, tl = divmod(tb, 4)
        ao = aos[tt % 2]
        aok = ('t1_ao', tt % 2)
        if tl == 0:
            p.dma('sp', ao[:], aov[:, :, tt * 512:(tt + 1) * 512], reads=[aoT_key], writes=[aok])
        xr = xrs[tb % 2]
        xk = ('t1_x', tb % 2)
        p.dma('sp', xr[:], xres[tb * 128:(tb + 1) * 128, :], writes=[xk])
        z = zs[tb % 2]
        zk = ('t1_z', tb % 2)
        for ct in range(4):
            pk, pb = p.bank()
            for kc in range(KC):
                p.mm(pb[:], ao[:, kc, tl * 128:(tl + 1) * 128], wres[:, kc, ct * 512:(ct + 1) * 512],
                     kc == 0, kc == KC - 1, [aok, 't1_w'], [pk])
            p.I('dve', 'scalar_tensor_tensor', [pk, xk], [zk], out=z[:, ct * 512:(ct + 1) * 512],
                in0=xr[:, ct * 512:(ct + 1) * 512], scalar=ALPHA, in1=pb[:], op0=ALU.mult, op1=ALU.add)
        emit_ln(p, z, zk, G, B, 't1', scr)
        p.dma('sp', xm_f32[tb * 128:(tb + 1) * 128, :], z[:], reads=[zk], writes=['xm_f32'])
        zbb = zb[tb % 2]
        zbk = ('t1_zb', tb % 2)
        p.I('act', 'copy', [zk], [zbk], out=zbb[:], in_=z[:])
        p.dma('sp', xm_bf[tb * 128:(tb + 1) * 128, :], zbb[:], reads=[zbk], writes=['xm_bf'])
        xt = xmT[tb % 2]
        xtk = ('t1_xmT', tb % 2)
        for q4 in range(4):
            pk, pb = p.bank()
            for j in range(4):
                kc = q4 * 4 + j
                p.tr(pb[:, j * 128:(j + 1) * 128], z[:, kc * 128:(kc + 1) * 128], c.identf[:], [zk, 'identf'], [pk])
            p.evac(xt[:, q4 * 4:(q4 + 1) * 4, :], pb[:].rearrange("q (k t) -> q k t", k=4), [pk], [xtk])
        pk, pb = p.bank()
        for kc in range(KC):
            p.mm(pb[:, 0:16], xt[:, kc, :], wr[:, kc, :], kc == 0, kc == KC - 1, [xtk, 'wr'], [pk])
        p.I('dve', 'tensor_tensor', [pk, 'rbb'], ['lg'], out=lg[:, tb, :], in0=pb[:, 0:16], in1=rbb[:], op=ALU.add)
    emit_routing(p, c, cd, idx_tab, RT)


def alloc_routing(p):
    RT = {}
    nb = NT // 128
    for nm, shp, dt in [('lg', [128, nb, 16], F32), ('ex', [128, nb, 16], F32), ('t16a', [128, nb, 16], F32),
                        ('t16b', [128, nb, 16], F32), ('sel', [128, nb, 16], F32), ('gate', [128, nb, 16], F32),
                        ('pos', [128, nb, 16], F32), ('m', [128, nb], F32), ('m1', [128, nb * 4], F32),
                        ('m2', [128, nb * 4], F32), ('sc', [128, nb * 4], F32), ('gs', [128, nb * 4], F32),
                        ('selb', [128, nb, 16], BF16), ('carry', [128, 16], F32), ('idxf', [128, nb, 16], F32),
                        ('eA', [128, nb], F32), ('eB', [128, nb], F32), ('posA', [128, nb], F32),
                        ('posB', [128, nb], F32), ('slotA', [128, nb], F32), ('slotB', [128, nb], F32),
                        ('gA', [128, nb], F32), ('gB', [128, nb], F32), ('okA', [128, nb], F32),
                        ('okB', [128, nb], F32), ('slotAi', [128, nb], I32), ('slotBi', [128, nb], I32),
                        ('tokid', [128, nb], I32), ('fill', [128, NSLOT // 128], I32),
                        ('ustr', [128, 128], BF16), ('onesb', [128, 128], BF16), ('ustrf', [128, 128], F32)]:
        RT[nm] = p.sb(shp, dt, 'rt_' + nm)
    return RT


def emit_routing(p, c, cd, idx_tab, RT):
    nb = NT // 128
    R = RT
    K = 'rt'

    def dv(method, **kw):
        p.I('dve', method, [K, 'lg'], [K], **kw)

    p.dma('sp', R['idxf'][:], cd['idxf'], writes=[K])
    p.dma('sp', R['tokid'][:], cd['tokid'], writes=[K])
    p.dma('sp', R['ustrf'][:], cd['ustrict'], writes=[K])
    p.dma('sp', R['fill'][:], cd['fill'], writes=[K])
    p.dma('sp', idx_tab.rearrange("(q j) o -> q (j o)", q=128), R['fill'][:], reads=[K], writes=['idx_init'])
    dv('tensor_copy', out=R['ustr'][:], in_=R['ustrf'][:])
    dv('memset', ap=R['onesb'][:], constant=1.0)
    lg, ex = R['lg'], R['ex']
    dv('tensor_reduce', out=R['m'][:], in_=lg[:], axis=AX.X, op=ALU.max)
    dv('tensor_tensor', out=ex[:], in0=lg[:], in1=R['m'][:].unsqueeze(2).to_broadcast([128, nb, 16]), op=ALU.subtract)
    p.I('act', 'activation', [K], [K], out=ex[:], in_=ex[:], func=AF.Exp)
    ex3 = ex[:].rearrange("q b (g i) -> q (b g) i", g=4)
    a3 = R['t16a'][:].rearrange("q b (g i) -> q (b g) i", g=4)
    b3 = R['t16b'][:].rearrange("q b (g i) -> q (b g) i", g=4)
    bc4 = lambda t: t[:].unsqueeze(2).to_broadcast([128, nb * 4, 4])
    dv('tensor_reduce', out=R['m1'][:], in_=ex3, axis=AX.X, op=ALU.max)
    dv('tensor_tensor', out=a3, in0=ex3, in1=bc4(R['m1']), op=ALU.is_equal)
    dv('scalar_tensor_tensor', out=b3, in0=a3, scalar=-1.0e30, in1=ex3, op0=ALU.mult, op1=ALU.add)
    dv('tensor_reduce', out=R['m2'][:], in_=b3, axis=AX.X, op=ALU.max)
    dv('tensor_tensor', out=R['sc'][:], in0=R['m1'][:], in1=R['m2'][:], op=ALU.add)
    sc3 = R['sc'][:].rearrange("q (b g) -> q b g", g=4)
    dv('tensor_reduce', out=R['m'][:], in_=sc3, axis=AX.X, op=ALU.max)
    dv('tensor_tensor', out=R['gs'][:].rearrange("q (b g) -> q b g", g=4), in0=sc3,
       in1=R['m'][:].unsqueeze(2).to_broadcast([128, nb, 4]), op=ALU.is_equal)
    dv('tensor_tensor', out=a3, in0=ex3, in1=bc4(R['m2']), op=ALU.is_ge)
    sel3 = R['sel'][:].rearrange("q b (g i) -> q (b g) i", g=4)
    dv('tensor_tensor', out=sel3, in0=a3, in1=bc4(R['gs']), op=ALU.mult)
    dv('tensor_copy', out=R['selb'][:], in_=R['sel'][:])
    dv('tensor_tensor', out=R['t16a'][:], in0=R['sel'][:], in1=ex[:], op=ALU.mult)
    dv('tensor_reduce', out=R['m'][:], in_=R['t16a'][:], axis=AX.X, op=ALU.add)
    dv('reciprocal', out=R['m'][:], in_=R['m'][:])
    dv('tensor_tensor', out=R['gate'][:], in0=R['t16a'][:],
       in1=R['m'][:].unsqueeze(2).to_broadcast([128, nb, 16]), op=ALU.mult)
    dv('memset', ap=R['carry'][:], constant=0.0)
    for b in range(nb):
        pk, pb = p.bank()
        p.mm(pb[:, 0:16], R['ustr'][:], R['selb'][:, b, :], True, True, [K], [pk])
        p.mm(pb[:, 16:32], R['onesb'][:], R['selb'][:, b, :], True, True, [K], [pk])
        p.I('dve', 'tensor_tensor', [pk, K], [K], out=R['pos'][:, b, :], in0=pb[:, 0:16], in1=R['carry'][:], op=ALU.add)
        p.I('dve', 'tensor_tensor', [pk, K], [K], out=R['carry'][:], in0=pb[:, 16:32], in1=R['carry'][:], op=ALU.add)
    idxf = R['idxf']
    bc16 = lambda t: t[:].unsqueeze(2).to_broadcast([128, nb, 16])
    dv('tensor_tensor', out=R['t16a'][:], in0=R['sel'][:], in1=idxf[:], op=ALU.mult)
    dv('tensor_reduce', out=R['eB'][:], in_=R['t16a'][:], axis=AX.X, op=ALU.max)
    dv('tensor_scalar', out=R['t16b'][:], in0=idxf[:], scalar1=-1.0, scalar2=15.0, op0=ALU.mult, op1=ALU.add)
    dv('tensor_tensor', out=R['t16b'][:], in0=R['t16b'][:], in1=R['sel'][:], op=ALU.mult)
    dv('tensor_reduce', out=R['eA'][:], in_=R['t16b'][:], axis=AX.X, op=ALU.max)
    dv('tensor_scalar', out=R['eA'][:], in0=R['eA'][:], scalar1=-1.0, scalar2=15.0, op0=ALU.mult, op1=ALU.add)
    dv('scalar_tensor_tensor', out=R['t16b'][:], in0=idxf[:], scalar=float(CAP), in1=R['pos'][:], op0=ALU.mult, op1=ALU.add)
    for s in ('A', 'B'):
        dv('tensor_tensor', out=R['t16a'][:], in0=idxf[:], in1=bc16(R['e' + s]), op=ALU.is_equal)
        dv('tensor_tensor', out=R['ex'][:], in0=R['t16a'][:], in1=R['pos'][:], op=ALU.mult)
        dv('tensor_reduce', out=R['pos' + s][:], in_=R['ex'][:], axis=AX.X, op=ALU.add)
        dv('tensor_tensor', out=R['ex'][:], in0=R['t16a'][:], in1=R['t16b'][:], op=ALU.mult)
        dv('tensor_reduce', out=R['slot' + s][:], in_=R['ex'][:], axis=AX.X, op=ALU.add)
        dv('tensor_tensor', out=R['ex'][:], in0=R['t16a'][:], in1=R['gate'][:], op=ALU.mult)
        dv('tensor_reduce', out=R['g' + s][:], in_=R['ex'][:], axis=AX.X, op=ALU.add)
        dv('tensor_scalar', out=R['ok' + s][:], in0=R['pos' + s][:], scalar1=float(CAP), scalar2=None, op0=ALU.is_lt)
        dv('tensor_tensor', out=R['g' + s][:], in0=R['g' + s][:], in1=R['ok' + s][:], op=ALU.mult)
        dv('tensor_scalar', out=R['ok' + s][:], in0=R['ok' + s][:], scalar1=-BIGSLOT, scalar2=BIGSLOT, op0=ALU.mult, op1=ALU.add)
        dv('tensor_tensor', out=R['slot' + s][:], in0=R['slot' + s][:], in1=R['ok' + s][:], op=ALU.add)
        dv('tensor_copy', out=R['slot' + s + 'i'][:], in_=R['slot' + s][:])
    n = 0
    for b in range(nb):
        for s in ('A', 'B'):
            p.scatter(idx_tab, R['slot' + s + 'i'][:, b:b + 1], R['tokid'][:, b:b + 1], NSLOT - 1,
                      [K, 'idx_init'], [('idx_w', n)], key=('idx_sc', n % 4))
            n += 1
    RT['idx_keys'] = [('idx_w', i) for i in range(n)] + ['idx_init']


def stage_moe(p, c, xm_bf, idx_tab, RT, wg, wu, wd, yslots):
    nsb = CAP // 128
    nch = [(0, 512), (512, CAP)] if CAP > 512 else [(0, CAP)]
    xg = [p.sb([128, D], BF16, f'me_xg{i}') for i in range(2)]
    idxs = [p.sb([128, 1], I32, f'me_ix{i}') for i in range(4)]
    xTe = [p.sb([128, KC, CAP], BF16, f'me_xT{i}') for i in range(2)]
    hT = p.sb([128, KC, CAP], BF16, 'me_hT')
    wts = [p.sb([128, KC, 512], BF16, f'me_w{i}') for i in range(5)]
    sil = [p.sb([128, CAP], BF16, f'me_sil{i}') for i in range(2)]
    yst = [p.sb([128, 512], F32, f'me_y{i}') for i in range(4)]
    for i in range(2):
        p.I('dve', 'memset', [], [('me_xg', i)], ap=xg[i][:], constant=0.0)
    wi = 0
    gi = 0
    yi = 0

    def wload(src):
        nonlocal wi
        w = wts[wi % 5]
        wk = ('me_w', wi % 5)
        wi += 1
        p.dma('pool', w[:], src.rearrange("(kc q) f -> q kc f", q=128), writes=[wk], key=('me_wld', wk))
        return w, wk

    for e in range(16):
        xT = xTe[e % 2]
        xTk = ('me_xT', e % 2)
        for sb in range(nsb):
            ix = idxs[gi % 4]
            ixk = ('me_ix', gi % 4)
            g = xg[gi % 2]
            gk = ('me_xg', gi % 2)
            gi += 1
            r0 = e * CAP + sb * 128
            p.dma('sp', ix[:], idx_tab[r0:r0 + 128, :], reads=RT['idx_keys'], writes=[ixk])
            p.gather(g[:], xm_bf, ix[:, 0:1], NT, [ixk, 'xm_bf'], [gk])
            for half in range(2):
                pk, pb = p.bank()
                pbv = pb[:].bitcast(BF16)
                for j in range(8):
                    kc = half * 8 + j
                    p.tr(pbv[:, j * 128:(j + 1) * 128], g[:, kc * 128:(kc + 1) * 128], c.identb[:], [gk, 'identb'], [pk])
                p.evac(xT[:, half * 8:(half + 1) * 8, sb * 128:(sb + 1) * 128],
                       pbv.rearrange("q (k t) -> q k t", k=8), [pk], [xTk])
        for ft in range(4):
            wgt, wgk = wload(wg[e, :, ft * 512:(ft + 1) * 512])
            wut, wuk = wload(wu[e, :, ft * 512:(ft + 1) * 512])
            for fb in range(4):
                fc = ft * 4 + fb
                gb = [p.bank() for _ in nch]
                ub = [p.bank() for _ in nch]
                for ci, (c0, c1) in enumerate(nch):
                    for kc in range(KC):
                        p.mm(gb[ci][1][:, 0:c1 - c0], wgt[:, kc, fb * 128:(fb + 1) * 128], xT[:, kc, c0:c1],
                             kc == 0, kc == KC - 1, [wgk, xTk], [gb[ci][0]])
                for ci, (c0, c1) in enumerate(nch):
                    for kc in range(KC):
                        p.mm(ub[ci][1][:, 0:c1 - c0], wut[:, kc, fb * 128:(fb + 1) * 128], xT[:, kc, c0:c1],
                             kc == 0, kc == KC - 1, [wuk, xTk], [ub[ci][0]])
                s = sil[fc % 2]
                sk = ('me_sil', fc % 2)
                for ci, (c0, c1) in enumerate(nch):
                    p.I('act', 'activation', [gb[ci][0]], [sk], out=s[:, c0:c1], in_=gb[ci][1][:, 0:c1 - c0], func=AF.Silu)
                for ci, (c0, c1) in enumerate(nch):
                    p.I('dve', 'tensor_tensor', [ub[ci][0], sk], ['me_hT'], out=hT[:, fc, c0:c1], in0=ub[ci][1][:, 0:c1 - c0],
                        in1=s[:, c0:c1], op=ALU.mult)
        for ct in range(4):
            wdt, wdk = wload(wd[e, :, ct * 512:(ct + 1) * 512])
            for sb in range(nsb):
                pk, pb = p.bank()
                for fc in range(KC):
                    p.mm(pb[:], hT[:, fc, sb * 128:(sb + 1) * 128], wdt[:, fc, :], fc == 0, fc == KC - 1,
                         ['me_hT', wdk], [pk])
                y = yst[yi % 4]
                yk = ('me_y', yi % 4)
                yi += 1
                p.evac(y[:], pb[:], [pk], [yk])
                r0 = e * CAP + sb * 128
                p.dma('sp', yslots[r0:r0 + 128, ct * 512:(ct + 1) * 512], y[:], reads=[yk], writes=['yslots'])


def stage_tail3(p, c, xm_f32, yslots, RT, lng, lnb, out_rows, out_key, xT_out=None):
    G = p.sb([128, D], F32, 't3_G')
    B = p.sb([128, D], F32, 't3_B')
    p.dma('sp', G[:], lng.partition_broadcast(128), writes=['lnG'])
    p.dma('sp', B[:], lnb.partition_broadcast(128), writes=['lnB'])
    ya = [p.sb([128, D], F32, f't3_ya{i}') for i in range(2)]
    yb = [p.sb([128, D], F32, f't3_yb{i}') for i in range(2)]
    zs = [p.sb([128, D], F32, f't3_z{i}') for i in range(2)]
    for i in range(2):
        p.I('dve', 'memset', [], [('t3_ya', i)], ap=ya[i][:], constant=0.0)
        p.I('dve', 'memset', [], [('t3_yb', i)], ap=yb[i][:], constant=0.0)
    scr = ln_scratch(p, 't3')
    if xT_out is not None:
        zb = [p.sb([128, D], BF16, f't3_zb{i}') for i in range(2)]
        stg = [p.sb([128, KC, 512], BF16, f't3_stg{i}') for i in range(2)]
        xT_v = xT_out.rearrange("(kc q) t -> q kc t", q=128)
    nblk = NT // 128
    for tb in range(nblk):
        a, ak = ya[tb % 2], ('t3_ya', tb % 2)
        b, bk = yb[tb % 2], ('t3_yb', tb % 2)
        z, zk = zs[tb % 2], ('t3_z', tb % 2)
        p.gather(a[:], yslots, RT['slotAi'][:, tb:tb + 1], NSLOT - 1, ['rt', 'yslots'], [ak])
        p.gather(b[:], yslots, RT['slotBi'][:, tb:tb + 1], NSLOT - 1, ['rt', 'yslots'], [bk])
        p.dma('sp', z[:], xm_f32[tb * 128:(tb + 1) * 128, :], reads=['xm_f32'], writes=[zk])
        p.I('act', 'mul', [zk], [zk], out=z[:], in_=z[:], mul=ALPHA)
        p.I('dve', 'scalar_tensor_tensor', [zk, ak, 'rt'], [zk], out=z[:], in0=a[:], scalar=RT['gA'][:, tb:tb + 1],
            in1=z[:], op0=ALU.mult, op1=ALU.add)
        p.I('dve', 'scalar_tensor_tensor', [zk, bk, 'rt'], [zk], out=z[:], in0=b[:], scalar=RT['gB'][:, tb:tb + 1],
            in1=z[:], op0=ALU.mult, op1=ALU.add)
        emit_ln(p, z, zk, G, B, 't3', scr)
        p.dma('sp', out_rows[tb * 128:(tb + 1) * 128, :], z[:], reads=[zk], writes=[out_key])
        if xT_out is not None:
            zbb, zbk = zb[tb % 2], ('t3_zb', tb % 2)
            p.I('act', 'copy', [zk], [zbk], out=zbb[:], in_=z[:])
            tile_i = tb // 4
            s, sk = stg[tile_i % 2], ('t3_stg', tile_i % 2)
            for half in range(2):
                pk, pb = p.bank()
                pbv = pb[:].bitcast(BF16)
                for j in range(8):
                    kc = half * 8 + j
                    p.tr(pbv[:, j * 128:(j + 1) * 128], zbb[:, kc * 128:(kc + 1) * 128], c.identb[:], [zbk, 'identb'], [pk])
                p.evac(s[:, half * 8:(half + 1) * 8, (tb % 4) * 128:(tb % 4 + 1) * 128],
                       pbv.rearrange("q (k t) -> q k t", k=8), [pk], [sk])
            if tb % 4 == 3:
                p.dma('sp', xT_v[:, :, tile_i * 512:(tile_i + 1) * 512], s[:], reads=[sk], writes=['x1T'])


def const_inputs(nc):
    din = lambda n, s, d=F32: nc.dram_tensor(n, s, d, kind="ExternalInput").ap()
    nb = NT // 128
    return dict(ident=din('c_ident', [128, 128]), ustrict=din('c_ustrict', [128, 128]),
                idxf=din('c_idxf', [128, nb, 16]), tokid=din('c_tokid', [128, nb], I32),
                fill=din('c_fill', [128, NSLOT // 128], I32))


def const_arrays():
    nb = NT // 128
    us = np.triu(np.ones((128, 128), np.float32), 1)
    return dict(c_ident=np.eye(128, dtype=np.float32), c_ustrict=us,
                c_idxf=np.broadcast_to(np.arange(16, dtype=np.float32), (128, nb, 16)).copy(),
                c_tokid=(np.arange(nb, dtype=np.int32)[None, :] * 128 + np.arange(128, dtype=np.int32)[:, None]).astype(np.int32),
                c_fill=np.full((128, NSLOT // 128), NT, np.int32))


def build_A(stages=('tr', 'proj', 'attn', 't1', 'moe', 't3'), debug=False):
    nc = bass.Bass("TRN2", target_bir_lowering=False)
    din = lambda n, s, d=F32: nc.dram_tensor(n, s, d, kind="ExternalInput").ap()
    dout = lambda n, s, d=F32: nc.dram_tensor(n, s, d, kind="ExternalOutput").ap()
    dint = (lambda n, s, d=F32: nc.dram_tensor(n, s, d, kind="Internal").ap()) if not debug else dout
    xin = din('xin', [NTH, D])
    wqkv = din('wqkv', [D, 3 * D])
    w_o = din('w_o', [D, D])
    biasT = din('biasT', [128, 32, 256])
    cvec = din('cvec', [32])
    kvalid = din('kvalid', [128, NTH // 128])
    lng = din('lng', [2, D])
    lnb = din('lnb', [2, D])
    rw = din('rw', [D, 16])
    rb = din('rb', [16])
    wg = din('wg', [16, D, D])
    wu = din('wu', [16, D, D])
    wd = din('wd', [16, D, D])
    cd = const_inputs(nc)
    x1 = dout('x1', [NT, D])
    x1T = dout('x1T', [D, NT], BF16)
    xT = dint('xT', [D, NTH], BF16)
    QT = dint('QT', [D, NT], BF16)
    KT = dint('KT', [D, NTH], BF16)
    V = dint('V', [NTH, D], BF16)
    aoT = dint('aoT', [D, NT], BF16)
    xm_f32 = dint('xm_f32', [NT, D])
    xm_bf = dint('xm_bf', [NT + 1, D], BF16)
    idx_tab = dint('idx_tab', [NSLOT, 1], I32)
    yslots = dint('yslots', [NSLOT, D])
    p = Prog(nc)
    p.init_arena(206 * 1024)
    p.make_banks()
    c = load_consts(p, cd)
    RT = alloc_routing(p)
    if debug:
        dbg_rt = dout('dbg_rt', [128, 8, NT // 128])
    m0 = p.mark()
    if 'tr' in stages:
        stage_transpose(p, c, xin, NTH, xT, 'xT', 'tr')
        p.release(m0)
    if 'proj' in stages:
        wres = p.sb([128, KC, D], BF16, 'pj_w')
        bufs = ([p.sb([128, KC, 512], BF16, f'pj_x{i}') for i in range(2)],
                [p.sb([128, KC, 512], BF16, f'pj_o{i}') for i in range(2)])
        bufs_tm = (bufs[0], [p.sb([128, D], BF16, f'pj_ot{i}') for i in range(2)])
        load_wres(p, wres, 'pj_w', wqkv[:, 0:D], D)
        stage_proj(p, xT[:, HALO:], 'xT', NT, wres, 'pj_w', D, 'fm', QT, 'QT', 'pq', bufs)
        load_wres(p, wres, 'pj_w', wqkv[:, D:2 * D], D)
        stage_proj(p, xT, 'xT', NTH, wres, 'pj_w', D, 'fm', KT, 'KT', 'pk', bufs)
        load_wres(p, wres, 'pj_w', wqkv[:, 2 * D:3 * D], D)
        stage_proj(p, xT, 'xT', NTH, wres, 'pj_w', D, 'tm', V, 'V', 'pv', bufs_tm)
        p.release(m0)
    if 'attn' in stages:
        stage_attnA(p, c, QT, KT, V, kvalid, biasT, cvec, aoT)
        p.release(m0)
    if 't1' in stages:
        zrow = p.sb([1, D], BF16, 'zrow')
        p.I('dve', 'memset', [], ['zrow'], ap=zrow[:], constant=0.0)
        p.dma('sp', xm_bf[NT:NT + 1, :], zrow[:], reads=['zrow'], writes=['xm_bf'])
        stage_tail1(p, c, aoT, 'aoT', xin[HALO:, :], w_o, lng[0], lnb[0], rw, rb, cd, xm_f32, xm_bf, idx_tab, RT)
        if debug:
            for i, nm in enumerate(['eA', 'eB', 'gA', 'gB', 'posA', 'posB', 'slotA', 'slotB']):
                p.dma('sp', dbg_rt[:, i, :], RT[nm][:], reads=['rt'], writes=['dbg_rt'])
        p.release(m0)
    if 'moe' in stages:
        stage_moe(p, c, xm_bf, idx_tab, RT, wg, wu, wd, yslots)
        p.release(m0)
    if 't3' in stages:
        stage_tail3(p, c, xm_f32, yslots, RT, lng[1], lnb[1], x1, 'x1', xT_out=x1T)
    p.barrier()
    p.emit()
    return nc, p


def prep_A(inputs, core):
    b, j = divmod(core, 4)
    x = inputs['x']
    t0 = j * NT
    xin = np.zeros((NTH, D), np.float32)
    if j > 0:
        xin[:] = x[b, t0 - HALO:t0 + NT]
    else:
        xin[HALO:] = x[b, 0:NT]
    kv = np.ones((NTH,), np.float32)
    if j == 0:
        kv[:HALO] = 0.0
    kvalid = np.ascontiguousarray(kv.reshape(NTH // 128, 128).T)
    rbias = inputs['a_rel_bias'][0]
    s = np.arange(128)[:, None]
    t = np.arange(128)[None, :]
    i3 = np.minimum(t - s + 128, 128) + 128
    i4 = t - s + 128
    biasT = np.concatenate([rbias[:, i3], rbias[:, i4]], axis=2)
    biasT = np.ascontiguousarray(biasT.transpose(1, 0, 2))
    d = dict(xin=xin, wqkv=inputs['a_w_qkv'][0], w_o=inputs['a_w_o'][0], biasT=biasT,
             cvec=np.ascontiguousarray(rbias[:, 256]), kvalid=kvalid,
             lng=inputs['ln_gain'][0], lnb=inputs['ln_bias'][0], rw=inputs['router_w'], rb=inputs['router_b'],
             wg=inputs['moe_w_gate'][0], wu=inputs['moe_w_up'][0], wd=inputs['moe_w_down'][0])
    d.update(const_arrays())
    return d


NTOK = 2 * SEQ
QBLK = 512
B1_DBG = 9


def build_B1(nunits=4, nqt=SEQ // QBLK):
    nc = bass.Bass("TRN2", target_bir_lowering=False)
    din = lambda n, s, d=F32: nc.dram_tensor(n, s, d, kind="ExternalInput").ap()
    dout = lambda n, s, d=F32: nc.dram_tensor(n, s, d, kind="ExternalOutput").ap()
    x1T = din('x1T', [D, NTOK], BF16)
    wq2 = din('wq2', [D, 256])
    wk2 = din('wk2', [D, 256])
    wv2 = din('wv2', [D, 256])
    wf2 = din('wf2', [D, 16])
    fgb = din('fgb', [2])
    tri_d = din('c_tri', [128, 128])
    trim_d = din('c_trimask', [128, 128])
    aoT2 = dout('aoT2', [256, NTOK], BF16)
    p = Prog(nc)
    p.init_arena(206 * 1024)
    p.make_banks()
    wq = p.sb([128, KC, 256], BF16, 'b1_wq')
    wk = p.sb([128, KC, 256], BF16, 'b1_wk')
    wv = p.sb([128, KC, 256], BF16, 'b1_wv')
    wf = p.sb([128, KC, 16], BF16, 'b1_wf')
    for w, src, nm in ((wq, wq2, 'b1_wq'), (wk, wk2, 'b1_wk'), (wv, wv2, 'b1_wv'), (wf, wf2, 'b1_wf')):
        p.dma('pool', w[:], src.rearrange("(kc q) f -> q kc f", q=128), writes=[nm])
    trif = p.sb([128, 128], F32, 'b1_tri')
    onesf = p.sb([128, 128], F32, 'b1_onesf')
    onesb = p.sb([128, 128], BF16, 'b1_onesb')
    trimf = p.sb([128, 128], F32, 'b1_trimf')
    trimb = p.sb([128, 128], BF16, 'b1_trimb')
    nfgb = p.sb([128, 2], F32, 'b1_nfgb')
    p.dma('sp', trif[:], tri_d, writes=['tri'])
    p.dma('sp', trimf[:], trim_d, writes=['trimf'])
    p.dma('sp', nfgb[:], fgb.partition_broadcast(128), writes=['nfgb'])
    p.I('dve', 'tensor_scalar', ['nfgb'], ['nfgb'], out=nfgb[:], in0=nfgb[:], scalar1=-1.0, scalar2=None, op0=ALU.mult)
    p.I('dve', 'memset', [], ['onesf'], ap=onesf[:], constant=1.0)
    p.I('dve', 'memset', [], ['onesb'], ap=onesb[:], constant=1.0)
    p.I('dve', 'tensor_copy', ['trimf'], ['trimb'], out=trimb[:], in_=trimf[:])
    QTu = p.sb([128, SEQ], BF16, 'b1_Q')
    KTu = p.sb([128, SEQ], BF16, 'b1_K')
    Vu = p.sb([128, SEQ // 128, 128], BF16, 'b1_V')
    xts = [p.sb([128, KC, 512], BF16, f'b1_x{i}') for i in range(2)]
    nblk = SEQ // 128
    lfz = p.sb([128, nblk], F32, 'b1_lfz')
    lf = p.sb([128, nblk], F32, 'b1_lf')
    fin = p.sb([128, nblk], F32, 'b1_fin')
    tot = p.sb([128, nblk], F32, 'b1_tot')
    sc = [p.sb([128, nblk], F32, f'b1_sc{i}') for i in range(2)]
    offi = p.sb([128, nblk], F32, 'b1_offi')
    fneg = p.sb([128, nblk], F32, 'b1_fneg')
    nbs = [p.sb([128, nblk], F32, f'b1_nb{i}') for i in range(2)]
    pts = [p.sb([128, 512], BF16, f'b1_p{i}') for i in range(4)]
    rds = [p.sb([128, 512], F32, f'b1_rd{i}') for i in range(2)]
    osts = [p.sb([128, 512], BF16, f'b1_o{i}') for i in range(2)]
    x1T_v = x1T.rearrange("(kc q) t -> q kc t", q=128)
    scale = 1.0 / math.sqrt(128.0)
    sbank = 0
    pi = 0
    for u in range(nunits):
        b, hh = divmod(u, 2)
        for tt in range(SEQ // 512):
            xt = xts[tt % 2]
            xk = ('b1_x', tt % 2)
            g0 = b * SEQ + tt * 512
            p.dma('sp', xt[:], x1T_v[:, :, g0:g0 + 512], writes=[xk])
            for (wres, wkey, dst, dk) in ((wq, 'b1_wq', QTu, 'Q'), (wk, 'b1_wk', KTu, 'K')):
                pk, pb = p.bank()
                for kc in range(KC):
                    p.mm(pb[:], wres[:, kc, hh * 128:(hh + 1) * 128], xt[:, kc, :], kc == 0, kc == KC - 1, [wkey, xk], [pk])
                p.evac(dst[:, tt * 512:(tt + 1) * 512], pb[:], [pk], [dk])
            for tb in range(4 if B1_DBG >= 2 else 0):
                blk = tt * 4 + tb
                pk, pb = p.bank()
                for kc in range(KC):
                    p.mm(pb[:, 0:128], xt[:, kc, tb * 128:(tb + 1) * 128], wv[:, kc, hh * 128:(hh + 1) * 128],
                         kc == 0, kc == KC - 1, ['b1_wv', xk], [pk])
                for kc in range(KC if B1_DBG != 21 else 0):
                    p.mm(pb[:, 128:144], xt[:, kc, tb * 128:(tb + 1) * 128], wf[:, kc, :],
                         kc == 0, kc == KC - 1, ['b1_wf', xk], [pk])
                ev = ('act', 'dve')[blk % 2]
                p.evac(Vu[:, blk, :], pb[:, 0:128], [pk], ['V'], eng=ev)
                p.evac(lfz[:, blk:blk + 1], pb[:, 128 + hh:129 + hh], [pk], ['lfz'], eng=ev)
        if B1_DBG < 3:
            continue
        p.I('act', 'activation', ['lfz', 'nfgb'], ['lf'], out=lf[:], in_=lfz[:], func=AF.Exp, scale=-1.0, bias=nfgb[:, hh:hh + 1])
        p.I('act', 'activation', ['lf'], ['lf'], out=lf[:], in_=lf[:], func=AF.Ln, bias=1.0, scale=1.0)
        p.I('dve', 'tensor_scalar', ['lf'], ['lf'], out=lf[:], in0=lf[:], scalar1=-1.0, scalar2=None, op0=ALU.mult)
        pk, pb = p.bank()
        p.mm(pb[:, 0:nblk], trif[:], lf[:], True, True, ['tri', 'lf'], [pk])
        p.mm(pb[:, nblk:2 * nblk], onesf[:], lf[:], True, True, ['onesf', 'lf'], [pk])
        p.I('dve', 'tensor_copy', [pk], ['fin'], out=fin[:], in_=pb[:, 0:nblk])
        p.I('dve', 'tensor_copy', [pk], ['tot'], out=tot[:], in_=pb[:, nblk:2 * nblk])
        p.I('dve', 'tensor_copy', ['tot'], ['sc0'], out=sc[0][:], in_=tot[:])
        k = 1
        cur = 0
        while k < nblk:
            a, bb = sc[cur], sc[1 - cur]
            ak, bk = f'sc{cur}', f'sc{1 - cur}'
            p.I('dve', 'tensor_copy', [ak], [bk], out=bb[:, 0:k], in_=a[:, 0:k])
            p.I('dve', 'tensor_tensor', [ak], [bk], out=bb[:, k:nblk], in0=a[:, k:nblk], in1=a[:, 0:nblk - k], op=ALU.add)
            cur = 1 - cur
            k *= 2
        p.I('dve', 'tensor_copy', [f'sc{cur}'], ['offi'], out=offi[:], in_=sc[cur][:])
        p.I('dve', 'tensor_tensor', ['fin', 'offi'], ['fneg'], out=fneg[:], in0=fin[:], in1=offi[:], op=ALU.add)
        p.I('dve', 'tensor_tensor', ['fneg', 'tot'], ['fneg'], out=fneg[:], in0=tot[:], in1=fneg[:], op=ALU.subtract)
        for qt in range(nqt):
            nk = 4 * qt + 4
            nb = nbs[qt % 2]
            nbk = ('b1_nb', qt % 2)
            r = 4 * qt + 1
            p.I('dve', 'tensor_scalar', ['fneg', 'offi'], [nbk], out=nb[:, 0:nk], in0=fneg[:, 0:nk],
                scalar1=offi[:, r:r + 1], scalar2=None, op0=ALU.add)
            ok_, ob = ('ps', 4 + qt % 2), p.banks[4 + qt % 2]
            lk_, lb = ('ps', 6 + qt % 2), p.banks[6 + qt % 2]
            q0 = qt * 512
            for kb in range(nk):
                i = kb - 4 * qt
                c0 = i * 128 if i > 0 else 0
                sk_, sb_ = ('ps', sbank % 4), p.banks[sbank % 4]
                sbank += 1
                p.mm(sb_[:, c0:512], KTu[:, kb * 128:(kb + 1) * 128], QTu[:, q0 + c0:q0 + 512], True, True, ['K', 'Q'], [sk_])
                pt = pts[pi % 4]
                ptk = ('b1_p', pi % 4)
                pi += 1
                p.I('act', 'activation', [sk_, nbk], [ptk], out=pt[:, c0:512], in_=sb_[:, c0:512], func=AF.Exp,
                    scale=scale, bias=nb[:, kb:kb + 1])
                if i >= 0:
                    p.I('dve', 'tensor_tensor', [ptk, 'trimb'], [ptk], out=pt[:, c0:c0 + 128], in0=pt[:, c0:c0 + 128],
                        in1=trimb[:], op=ALU.mult)
                p.mm(ob[:, c0:512], Vu[:, kb, :], pt[:, c0:512], kb == 0, kb == nk - 1, ['V', ptk], [ok_])
                p.mm(lb[:, c0:512], onesb[:], pt[:, c0:512], kb == 0, kb == nk - 1, ['onesb', ptk], [lk_])
            rd = rds[qt % 2]
            rk = ('b1_rd', qt % 2)
            p.I('dve', 'reciprocal', [lk_], [rk], out=rd[:], in_=lb[:])
            ost = osts[qt % 2]
            osk = ('b1_o', qt % 2)
            p.I('dve', 'tensor_tensor', [ok_, rk], [osk], out=ost[:], in0=ob[:], in1=rd[:], op=ALU.mult)
            g0 = b * SEQ + q0
            p.dma('sp', aoT2[hh * 128:(hh + 1) * 128, g0:g0 + 512], ost[:], reads=[osk], writes=['aoT2'])
    p.barrier()
    p.emit()
    return nc, p


def prep_B1(inputs, core, x1T_full):
    c = core
    kvw = inputs['kv_w']
    wv = kvw[:, D + 256 * c:D + 256 * c + 256]
    wf = np.zeros((D, 16), np.float32)
    wf[:, 0:2] = inputs['fg_w'][:, 2 * c:2 * c + 2]
    tri = np.triu(np.ones((128, 128), np.float32), 0)
    return dict(x1T=x1T_full, wq2=np.ascontiguousarray(inputs['b_w_q'][0][:, 256 * c:256 * c + 256]),
                wk2=np.ascontiguousarray(kvw[:, 256 * c:256 * c + 256]), wv2=np.ascontiguousarray(wv), wf2=wf,
                fgb=np.ascontiguousarray(inputs['fg_b'][2 * c:2 * c + 2]), c_tri=tri, c_trimask=tri.copy())


def build_B2():
    nc = bass.Bass("TRN2", target_bir_lowering=False)
    din = lambda n, s, d=F32: nc.dram_tensor(n, s, d, kind="ExternalInput").ap()
    dout = lambda n, s, d=F32: nc.dram_tensor(n, s, d, kind="ExternalOutput").ap()
    dint = lambda n, s, d=F32: nc.dram_tensor(n, s, d, kind="Internal").ap()
    aoT = din('aoT', [D, NT], BF16)
    xres = din('xres', [NT, D])
    w_o = din('w_o', [D, D])
    lng = din('lng', [2, D])
    lnb = din('lnb', [2, D])
    rw = din('rw', [D, 16])
    rb = din('rb', [16])
    wg = din('wg', [16, D, D])
    wu = din('wu', [16, D, D])
    wd = din('wd', [16, D, D])
    cd = const_inputs(nc)
    out = dout('out', [NT, D])
    xm_f32 = dint('xm_f32', [NT, D])
    xm_bf = dint('xm_bf', [NT + 1, D], BF16)
    idx_tab = dint('idx_tab', [NSLOT, 1], I32)
    yslots = dint('yslots', [NSLOT, D])
    p = Prog(nc)
    p.init_arena(206 * 1024)
    p.make_banks()
    c = load_consts(p, cd)
    RT = alloc_routing(p)
    m0 = p.mark()
    zrow = p.sb([1, D], BF16, 'zrow')
    p.I('dve', 'memset', [], ['zrow'], ap=zrow[:], constant=0.0)
    p.dma('sp', xm_bf[NT:NT + 1, :], zrow[:], reads=['zrow'], writes=['xm_bf'])
    stage_tail1(p, c, aoT, 'aoT', xres, w_o, lng[0], lnb[0], rw, rb, cd, xm_f32, xm_bf, idx_tab, RT)
    p.release(m0)
    stage_moe(p, c, xm_bf, idx_tab, RT, wg, wu, wd, yslots)
    p.release(m0)
    stage_tail3(p, c, xm_f32, yslots, RT, lng[1], lnb[1], out, 'out')
    p.barrier()
    p.emit()
    return nc, p


def kernel(**inputs):
    inputs = {k: np.asarray(v) for k, v in inputs.items()}
    cores = list(range(NCORES))
    ncA, _ = build_A()
    resA = run_bass_kernel_spmd(ncA, [prep_A(inputs, c) for c in cores], core_ids=cores)
    x1 = [np.asarray(r['x1']) for r in resA.results]
    x1T_full = np.ascontiguousarray(np.concatenate([np.asarray(r['x1T']) for r in resA.results], axis=1))
    ncB1, _ = build_B1()
    resB1 = run_bass_kernel_spmd(ncB1, [prep_B1(inputs, c, x1T_full) for c in cores], core_ids=cores)
    aoT_full = np.concatenate([np.asarray(r['aoT2']) for r in resB1.results], axis=0)
    ncB2, _ = build_B2()
    cst = const_arrays()
    in2 = []
    for c in cores:
        d = dict(aoT=np.ascontiguousarray(aoT_full[:, c * NT:(c + 1) * NT]), xres=x1[c], w_o=inputs['b_w_o'][0],
                 lng=inputs['ln_gain'][1], lnb=inputs['ln_bias'][1], rw=inputs['router_w'], rb=inputs['router_b'],
                 wg=inputs['moe_w_gate'][1], wu=inputs['moe_w_up'][1], wd=inputs['moe_w_down'][1])
        d.update(cst)
        in2.append(d)
    resB2 = run_bass_kernel_spmd(ncB2, in2, core_ids=cores)
    out = np.stack([np.asarray(r['out']) for r in resB2.results], axis=0)
    return out.reshape(2, SEQ, D).astype(np.float32)
```

```python
import math
import numpy as np
import concourse.bass as bass
import concourse.mybir as mybir
from concourse.bass_utils import run_bass_kernel_spmd
from contextlib import ExitStack

F32 = mybir.dt.float32
BF16 = mybir.dt.bfloat16
I32 = mybir.dt.int32
ALU = mybir.AluOpType
AF = mybir.ActivationFunctionType
AX = mybir.AxisListType

D = 2048
KC = 16
NT = 4096
HALO = 512
NTH = NT + HALO
SEQ = 16384
NCORES = 8
CAP = 640
NSLOT = 16 * CAP
ALPHA = (2.0 * 2) ** 0.25
LN_EPS = 1e-5
BIGSLOT = 1.0e6

ENGS = ('pe', 'act', 'dve', 'pool', 'sp')
ROT = 30000


class Op:
    __slots__ = ('eng', 'fn', 'deps', 'signal', 'ms', 'dma_key', 'dma_val', 'is_dma')


class Prog:
    def __init__(self, nc):
        self.nc = nc
        self.ops = {e: [] for e in ENGS}
        self.last_w = {}
        self.readers = {}
        self.dma_cnt = {}
        self.stack = ExitStack()
        self.n_names = 0
        self.banks = None
        self.bank_i = 0
        self.ev_i = 0

    def init_arena(self, nbytes=200 * 1024):
        self.arena = self.stack.enter_context(self.nc.sbuf_tensor("arena", [128, nbytes // 4], F32))
        self.arena_top = 0
        self.arena_size = nbytes

    def sb(self, shape, dt, name=None):
        esz = {F32: 4, I32: 4, BF16: 2}[dt]
        nel = 1
        for d in shape[1:]:
            nel *= d
        nbytes = (nel * esz + 63) // 64 * 64
        off = self.arena_top
        assert off + nbytes <= self.arena_size, (name, off, nbytes)
        self.arena_top = off + nbytes
        v = self.arena[0:shape[0], off // 4:(off + nel * esz + 3) // 4]
        if dt != F32:
            v = v.bitcast(dt)
        if esz == 2 and nel % 2 == 1:
            v = v[:, 0:nel]
        if len(shape) == 3:
            v = v.rearrange("q (a b) -> q a b", a=shape[1])
        elif len(shape) == 4:
            v = v.rearrange("q (a b c) -> q a b c", a=shape[1], b=shape[2])
        return v

    def mark(self):
        return self.arena_top

    def release(self, m):
        self.barrier()
        self.arena_top = m

    def barrier(self):
        lasts = {}
        for e in ENGS:
            for op in reversed(self.ops[e]):
                if op.fn is not None and not op.is_dma:
                    lasts[e] = op
                    break
        dmas = [('dma', k, 16 * c) for k, c in self.dma_cnt.items()]
        for e in ENGS:
            op = Op()
            op.eng = e
            op.fn = None
            op.signal = False
            op.ms = None
            op.is_dma = False
            op.dma_key = None
            op.dma_val = None
            op.deps = list(dmas)
            for e2, l in lasts.items():
                if e2 != e:
                    l.signal = True
                    op.deps.append(('op', l))
            self.ops[e].append(op)
        self.last_w = {}
        self.readers = {}

    def make_banks(self):
        self.banks = []
        for i in range(8):
            self.banks.append(self.stack.enter_context(self.nc.psum_tensor(f"bank{i}", [128, 512], F32)))

    def bank(self):
        i = self.bank_i
        self.bank_i = (i + 1) % 8
        return ('ps', i), self.banks[i]

    def _add(self, eng, fn, reads, writes, is_dma=False, dma_key=None):
        op = Op()
        op.eng = eng
        op.fn = fn
        op.signal = False
        op.ms = None
        op.is_dma = is_dma
        op.dma_key = dma_key
        op.dma_val = None
        deps = {}
        for k in reads:
            w = self.last_w.get(k)
            if w is not None:
                deps[id(w)] = (w, 'RAW')
        for k in writes:
            w = self.last_w.get(k)
            if w is not None and id(w) not in deps:
                deps[id(w)] = (w, 'WAW')
            for r in self.readers.get(k, ()):
                if id(r) not in deps:
                    deps[id(r)] = (r, 'WAR')
        waits = []
        for d, kind in deps.values():
            if d.is_dma:
                waits.append(('dma', d.dma_key, d.dma_val))
            else:
                if d.eng == eng and not is_dma:
                    if eng == 'pe':
                        continue
                    if kind != 'RAW':
                        continue
                d.signal = True
                waits.append(('op', d))
        op.deps = waits
        if is_dma:
            c = self.dma_cnt.get(dma_key, 0) + 1
            self.dma_cnt[dma_key] = c
            op.dma_val = 16 * c
        for k in reads:
            self.readers.setdefault(k, []).append(op)
        for k in writes:
            self.last_w[k] = op
            self.readers[k] = []
        self.ops[eng].append(op)
        return op

    def I(self, eng, method, reads, writes, *a, **kw):
        return self._add(eng, lambda e: getattr(e, method)(*a, **kw), tuple(reads), tuple(writes))

    def mm(self, out, lhsT, rhs, start, stop, reads, writes, **kw):
        return self._add('pe', lambda e: e.matmul(out, lhsT=lhsT, rhs=rhs, start=start, stop=stop, **kw),
                         tuple(reads), tuple(writes))

    def tr(self, out, in_, ident, reads, writes):
        return self._add('pe', lambda e: e.transpose(out=out, in_=in_, identity=ident), tuple(reads), tuple(writes))

    def evac(self, out, in_, reads, writes, eng=None):
        if eng is None:
            eng = ('act', 'dve')[self.ev_i % 2]
            self.ev_i += 1
        if eng == 'act':
            return self.I('act', 'copy', reads, writes, out=out, in_=in_)
        return self.I(eng, 'tensor_copy', reads, writes, out=out, in_=in_)

    def dma(self, queue, out, in_, reads=(), writes=(), key=None, **kw):
        if key is None:
            key = ('dmak',) + tuple(writes)
        return self._add(queue, lambda e: e.dma_start(out=out, in_=in_, **kw),
                         tuple(reads), tuple(writes), is_dma=True, dma_key=key)

    def dma_fn(self, queue, fn, reads=(), writes=(), key=None):
        if key is None:
            key = ('dmak',) + tuple(writes)
        return self._add(queue, fn, tuple(reads), tuple(writes), is_dma=True, dma_key=key)

    def _bound_reg(self, e, bound):
        regs = self.__dict__.setdefault('bound_regs', {})
        if bound not in regs:
            r = e.alloc_register(f"bnd{bound}")
            e.reg_mov(r, bound)
            regs[bound] = r
        return regs[bound]

    def gather(self, out, table, idx_ap, bound, reads, writes, key=None):
        return self.dma_fn('pool', lambda e: e.indirect_dma_start(
            out=out, out_offset=None, in_=table,
            in_offset=bass.IndirectOffsetOnAxis(ap=idx_ap, axis=0),
            bounds_check=self._bound_reg(e, bound), oob_is_err=False), reads, writes, key)

    def scatter(self, table, idx_ap, src, bound, reads, writes, key=None):
        return self.dma_fn('pool', lambda e: e.indirect_dma_start(
            out=table, out_offset=bass.IndirectOffsetOnAxis(ap=idx_ap, axis=0),
            in_=src, in_offset=None, bounds_check=self._bound_reg(e, bound), oob_is_err=False), reads, writes, key)

    def fence(self, eng, keys):
        return self._add(eng, None, tuple(keys), ())

    def emit(self):
        nc = self.nc
        st = self.stack
        eng_sems = {e: [] for e in ENGS}
        for e in ENGS:
            n = 0
            for op in self.ops[e]:
                if op.is_dma or not op.signal:
                    continue
                si, v = divmod(n, ROT)
                n += 1
                while len(eng_sems[e]) <= si:
                    eng_sems[e].append(st.enter_context(nc.semaphore(f"s_{e}_{len(eng_sems[e])}")))
                op.ms = (eng_sems[e][si], v + 1)
        dma_sems = {}
        for i, k in enumerate(self.dma_cnt):
            assert 16 * self.dma_cnt[k] < 60000, (k, self.dma_cnt[k])
            dma_sems[k] = st.enter_context(nc.semaphore(f"s_dma_{i}"))
        self.n_sems = sum(len(v) for v in eng_sems.values()) + len(dma_sems)

        def run(e, eng):
            waited = {}
            for op in self.ops[e]:
                for w in op.deps:
                    if w[0] == 'dma':
                        sem, val = dma_sems[w[1]], w[2]
                    else:
                        sem, val = w[1].ms
                    if waited.get(id(sem), 0) >= val:
                        continue
                    waited[id(sem)] = val
                    eng.wait_ge(sem, val)
                if op.fn is None:
                    continue
                inst = op.fn(eng)
                if op.is_dma:
                    inst.then_inc(dma_sems[op.dma_key], 16)
                elif op.ms is not None:
                    inst.then_inc(op.ms[0], 1)

        with nc.Block() as block:
            @block.tensor
            def _(eng):
                run('pe', eng)

            @block.scalar
            def _(eng):
                run('act', eng)

            @block.vector
            def _(eng):
                run('dve', eng)

            @block.gpsimd
            def _(eng):
                run('pool', eng)

            @block.sync
            def _(eng):
                run('sp', eng)
        st.close()


class Consts:
    pass


def load_consts(p, cd):
    c = Consts()
    identf = p.sb([128, 128], F32, 'identf')
    p.dma('sp', identf[:], cd['ident'], writes=['identf'])
    identb = p.sb([128, 128], BF16, 'identb')
    p.I('dve', 'tensor_copy', ['identf'], ['identb'], out=identb[:], in_=identf[:])
    c.identf, c.identb = identf, identb
    return c


def stage_transpose(p, c, x_rows, ntok, xT_out, xT_key, tag):
    nblk = ntok // 128
    xb = [p.sb([128, D], BF16, f'{tag}_xb{i}') for i in range(2)]
    stg = [p.sb([128, KC, 512], BF16, f'{tag}_stg{i}') for i in range(2)]
    xT_v = xT_out.rearrange("(kc q) t -> q kc t", q=128)
    for tb in range(nblk):
        b = xb[tb % 2]
        bk = (tag, 'xb', tb % 2)
        p.dma('pool', b[:], x_rows[tb * 128:(tb + 1) * 128, :], writes=[bk])
        tile_i = tb // 4
        s = stg[tile_i % 2]
        sk = (tag, 'stg', tile_i % 2)
        for half in range(2):
            pk, pb = p.bank()
            pbv = pb[:].bitcast(BF16)
            for j in range(8):
                kc = half * 8 + j
                p.tr(pbv[:, j * 128:(j + 1) * 128], b[:, kc * 128:(kc + 1) * 128], c.identb[:],
                     [bk, 'identb'], [pk])
            p.evac(s[:, half * 8:(half + 1) * 8, (tb % 4) * 128:(tb % 4 + 1) * 128],
                   pbv.rearrange("q (k t) -> q k t", k=8), [pk], [sk])
        if tb % 4 == 3:
            p.dma('sp', xT_v[:, :, tile_i * 512:(tile_i + 1) * 512], s[:], reads=[sk], writes=[xT_key])


def load_wres(p, wres, wkey, w_ap, F):
    wv = w_ap.rearrange("(kc q) f -> q kc f", q=128)
    for c0 in range(0, F, 512):
        c1 = min(F, c0 + 512)
        p.dma('pool', wres[:, :, c0:c1], wv[:, :, c0:c1], writes=[wkey], key=('wld', wkey))


def stage_proj(p, xT_in, xT_key, ntok, wres, wkey, F, mode, out, out_key, tag, bufs):
    xts, osts = bufs
    xT_v = xT_in.rearrange("(kc q) t -> q kc t", q=128)
    ntile = ntok // 512
    for tt in range(ntile):
        xt = xts[tt % 2]
        xk = (tag, 'xt', tt % 2)
        p.dma('sp', xt[:], xT_v[:, :, tt * 512:(tt + 1) * 512], reads=[xT_key], writes=[xk])
        if mode == 'fm':
            ost = osts[tt % 2]
            ok = (tag, 'ost', tt % 2)
            nfc = F // 128
            for fc in range(nfc):
                pk, pb = p.bank()
                for kc in range(KC):
                    p.mm(pb[:], wres[:, kc, fc * 128:(fc + 1) * 128], xt[:, kc, :], kc == 0, kc == KC - 1,
                         [wkey, xk], [pk])
                p.evac(ost[:, fc, :], pb[:], [pk], [ok])
            ov = out.rearrange("(fc q) t -> q fc t", q=128)
            p.dma('sp', ov[:, :, tt * 512:(tt + 1) * 512], ost[:, 0:nfc, :], reads=[ok], writes=[out_key])
        else:
            for tb in range(4):
                ost = osts[(tt * 4 + tb) % 2]
                ok = (tag, 'ost', (tt * 4 + tb) % 2)
                for c0 in range(0, F, 512):
                    c1 = min(F, c0 + 512)
                    pk, pb = p.bank()
                    for kc in range(KC):
                        p.mm(pb[:, 0:c1 - c0], xt[:, kc, tb * 128:(tb + 1) * 128], wres[:, kc, c0:c1],
                             kc == 0, kc == KC - 1, [wkey, xk], [pk])
                    p.evac(ost[:, c0:c1], pb[:, 0:c1 - c0], [pk], [ok])
                r0 = tt * 512 + tb * 128
                p.dma('sp', out[r0:r0 + 128, 0:F], ost[:, 0:F], reads=[ok], writes=[out_key])


def stage_attnA(p, c, QT, KT, V, kvalid_d, biasT_d, cvec_d, aoT, tag='aa'):
    etab = p.sb([128, 32, 256], BF16, 'etab')
    btmp = [p.sb([128, 8, 256], F32, f'btmp{i}') for i in range(1)]
    for g in range(4):
        bt = btmp[0]
        bk = ('btmp', 0)
        p.dma('sp', bt[:], biasT_d[:, g * 8:(g + 1) * 8, :], writes=[bk])
        p.I('act', 'activation', [bk], ['etab'], out=etab[:, g * 8:(g + 1) * 8, :], in_=bt[:], func=AF.Exp)
    p.I('dve', 'memset', [], ['etab'], etab[64:128, :, 128:192], 0.0)
    cb = p.sb([128, 32], F32, 'cbias')
    p.dma('sp', cb[:], cvec_d.partition_broadcast(128), writes=['cbias'])
    kval = p.sb([128, NTH // 128], F32, 'kval')
    p.dma('sp', kval[:], kvalid_d, writes=['kval'])
    nkb = NTH // 128
    vones = p.sb([128, nkb, 64], BF16, 'vones')
    p.I('dve', 'tensor_copy', ['kval'], ['vones'], out=vones[:],
        in_=kval[:].unsqueeze(2).to_broadcast([128, nkb, 64]))

    QT_v = QT.rearrange("(kc q) t -> q kc t", q=128)
    KT_v = KT.rearrange("(kc q) t -> q kc t", q=128)
    V_v = V.rearrange("(b q) f -> q b f", q=128)
    aoT_v = aoT.rearrange("(kc q) t -> q kc t", q=128)
    qts = [p.sb([128, KC, 512], BF16, f'aa_q{i}') for i in range(1)]
    kts = [p.sb([128, KC, 512], BF16, f'aa_k{i}') for i in range(3)]
    vts = [p.sb([128, 4, D], BF16, f'aa_v{i}') for i in range(3)]
    aos = [p.sb([128, KC, 512], BF16, f'aa_o{i}') for i in range(2)]
    pts = [p.sb([128, 640], BF16, f'aa_p{i}') for i in range(3)]
    rdn = [p.sb([128, 128], F32, f'aa_r{i}') for i in range(2)]

    def load_kv(j):
        p.dma('sp', kts[j % 3][:], KT_v[:, :, j * 512:(j + 1) * 512], reads=['KT'], writes=[('aa_k', j % 3)])
        p.dma('sp', vts[j % 3][:], V_v[:, j * 4:(j + 1) * 4, :], reads=['V'], writes=[('aa_v', j % 3)])

    load_kv(0)
    load_kv(1)
    ntile = NT // 512
    hp = 0
    for tt in range(ntile):
        if tt + 2 <= ntile:
            load_kv(tt + 2)
        qt = qts[0]
        qk = ('aa_q', 0)
        p.dma('sp', qt[:], QT_v[:, :, tt * 512:(tt + 1) * 512], reads=['QT'], writes=[qk])
        ao = aos[tt % 2]
        aok = ('aa_o', tt % 2)
        tasks = [(qi, pr, hh) for qi in range(4) for pr in range(16) for hh in range(2)]
        st = {}

        def qk_part(task):
            qi, pr, hh = task
            pb0 = hh * 64
            xk_, xb_ = p.bank()
            yk_, yb_ = p.bank()
            blks = []
            for kb in range(5):
                w = qi + kb
                j = tt + (w // 4)
                blk = w % 4
                blks.append((j, blk))
                dst = xb_[:, kb * 128:(kb + 1) * 128] if kb < 3 else yb_[:, (kb - 3) * 128:(kb - 2) * 128]
                p.mm(dst, kts[j % 3][pb0:pb0 + 64, pr, blk * 128:(blk + 1) * 128],
                     qt[pb0:pb0 + 64, pr, qi * 128:(qi + 1) * 128], True, True,
                     [('aa_k', j % 3), qk], [xk_ if kb < 3 else yk_])
            st[task] = (xk_, xb_, yk_, yb_, blks)

        def rest_part(task):
            nonlocal hp
            qi, pr, hh = task
            h = pr * 2 + hh
            pb0 = hh * 64
            xk_, xb_, yk_, yb_, blks = st.pop(task)
            if hh == 0:
                st[('ob', qi, pr)] = p.bank()
            ok_, ob = st[('ob', qi, pr)]
            pt = pts[hp % 3]
            ptk = ('aa_p', hp % 3)
            hp += 1
            p.I('act', 'activation', [xk_, 'cbias'], [ptk], out=pt[:, 0:384], in_=xb_[:, 0:384],
                func=AF.Exp, bias=cb[:, h:h + 1], scale=0.125)
            p.I('act', 'activation', [yk_], [ptk], out=pt[:, 384:640], in_=yb_[:, 0:256],
                func=AF.Exp, scale=0.125)
            p.I('pool', 'memset', [ptk], [ptk], pt[0:64, 64:128], 0.0)
            p.I('dve', 'tensor_tensor', [ptk, 'etab'], [ptk], out=pt[:, 384:640], in0=pt[:, 384:640],
                in1=etab[:, h, :], op=ALU.mult)
            for kb in range(5):
                j, blk = blks[kb]
                p.mm(ob[pb0:pb0 + 64, 0:128], vts[j % 3][:, blk, h * 64:(h + 1) * 64],
                     pt[:, kb * 128:(kb + 1) * 128], kb == 0, kb == 4,
                     [('aa_v', j % 3), ptk], [ok_], tile_position=(0, pb0))
            for kb in range(5):
                j, blk = blks[kb]
                p.mm(ob[pb0:pb0 + 64, 128:256], vones[:, j * 4 + blk, :],
                     pt[:, kb * 128:(kb + 1) * 128], kb == 0, kb == 4,
                     ['vones', ptk], [ok_], tile_position=(0, pb0))
            if hh == 1:
                del st[('ob', qi, pr)]
                rd = rdn[pr % 2]
                rk = ('aa_r', pr % 2)
                p.I('dve', 'reciprocal', [ok_], [rk], out=rd[:], in_=ob[:, 128:256])
                p.I('dve', 'tensor_tensor', [ok_, rk], [aok], out=ao[:, pr, qi * 128:(qi + 1) * 128],
                    in0=ob[:, 0:128], in1=rd[:], op=ALU.mult)

        LA = 1
        for i in range(len(tasks) + LA):
            if i < len(tasks):
                qk_part(tasks[i])
            if i >= LA:
                rest_part(tasks[i - LA])
        p.dma('sp', aoT_v[:, :, tt * 512:(tt + 1) * 512], ao[:], reads=[aok], writes=['aoT'])


def emit_ln(p, z, zk, G, B, tag, scr):
    stats, mv, rstd = scr
    sk = (tag, 'lnscr')
    for ch in range(4):
        p.I('dve', 'bn_stats', [zk], [sk], out=stats[:, ch, :], in_=z[:, ch * 512:(ch + 1) * 512])
    p.I('dve', 'bn_aggr', [sk], [sk], out=mv[:], in_=stats[:])
    p.I('act', 'activation', [sk], [sk], out=rstd[:], in_=mv[:, 1:2], func=AF.Sqrt, bias=LN_EPS, scale=1.0)
    p.I('dve', 'reciprocal', [sk], [sk], out=rstd[:], in_=rstd[:])
    p.I('dve', 'tensor_scalar', [zk, sk], [zk], out=z[:], in0=z[:], scalar1=mv[:, 0:1], scalar2=rstd[:],
        op0=ALU.subtract, op1=ALU.mult)
    p.I('pool', 'tensor_tensor', [zk, 'lnG'], [zk], out=z[:], in0=z[:], in1=G[:], op=ALU.mult)
    p.I('pool', 'tensor_tensor', [zk, 'lnB'], [zk], out=z[:], in0=z[:], in1=B[:], op=ALU.add)


def ln_scratch(p, tag):
    return (p.sb([128, 4, 6], F32, f'{tag}_st'), p.sb([128, 2], F32, f'{tag}_mv'), p.sb([128, 1], F32, f'{tag}_rs'))


def stage_tail1(p, c, aoT, aoT_key, xres, w_o, lng, lnb, rw, rb, cd, xm_f32, xm_bf, idx_tab, RT):
    wres = p.sb([128, KC, D], BF16, 't1_w')
    load_wres(p, wres, 't1_w', w_o, D)
    G = p.sb([128, D], F32, 't1_G')
    B = p.sb([128, D], F32, 't1_B')
    p.dma('sp', G[:], lng.partition_broadcast(128), writes=['lnG'])
    p.dma('sp', B[:], lnb.partition_broadcast(128), writes=['lnB'])
    wr = p.sb([128, KC, 16], F32, 't1_wr')
    p.dma('sp', wr[:], rw.rearrange("(kc q) e -> q kc e", q=128), writes=['wr'])
    rbb = p.sb([128, 16], F32, 't1_rb')
    p.dma('sp', rbb[:], rb.partition_broadcast(128), writes=['rbb'])
    aov = aoT.rearrange("(kc q) t -> q kc t", q=128)
    aos = [p.sb([128, KC, 512], BF16, f't1_ao{i}') for i in range(2)]
    xrs = [p.sb([128, D], F32, f't1_x{i}') for i in range(2)]
    zs = [p.sb([128, D], F32, f't1_z{i}') for i in range(2)]
    zb = [p.sb([128, D], BF16, f't1_zb{i}') for i in range(2)]
    xmT = [p.sb([128, KC, 128], F32, f't1_xmT{i}') for i in range(2)]
    scr = ln_scratch(p, 't1')
    lg = RT['lg']
    nblk = NT // 128
    for tb in range(nblk):
        tt, tl = divmod(tb, 4)
        ao = aos[tt % 2]
        aok = ('t1_ao', tt % 2)
        if tl == 0:
            p.dma('sp', ao[:], aov[:, :, tt * 512:(tt + 1) * 512], reads=[aoT_key], writes=[aok])
        xr = xrs[tb % 2]
        xk = ('t1_x', tb % 2)
        p.dma('sp', xr[:], xres[tb * 128:(tb + 1) * 128, :], writes=[xk])
        z = zs[tb % 2]
        zk = ('t1_z', tb % 2)
        for ct in range(4):
            pk, pb = p.bank()
            for kc in range(KC):
                p.mm(pb[:], ao[:, kc, tl * 128:(tl + 1) * 128], wres[:, kc, ct * 512:(ct + 1) * 512],
                     kc == 0, kc == KC - 1, [aok, 't1_w'], [pk])
            p.I('dve', 'scalar_tensor_tensor', [pk, xk], [zk], out=z[:, ct * 512:(ct + 1) * 512],
                in0=xr[:, ct * 512:(ct + 1) * 512], scalar=ALPHA, in1=pb[:], op0=ALU.mult, op1=ALU.add)
        emit_ln(p, z, zk, G, B, 't1', scr)
        p.dma('sp', xm_f32[tb * 128:(tb + 1) * 128, :], z[:], reads=[zk], writes=['xm_f32'])
        zbb = zb[tb % 2]
        zbk = ('t1_zb', tb % 2)
        p.I('act', 'copy', [zk], [zbk], out=zbb[:], in_=z[:])
        p.dma('sp', xm_bf[tb * 128:(tb + 1) * 128, :], zbb[:], reads=[zbk], writes=['xm_bf'])
        xt = xmT[tb % 2]
        xtk = ('t1_xmT', tb % 2)
        for q4 in range(4):
            pk, pb = p.bank()
            for j in range(4):
                kc = q4 * 4 + j
                p.tr(pb[:, j * 128:(j + 1) * 128], z[:, kc * 128:(kc + 1) * 128], c.identf[:], [zk, 'identf'], [pk])
            p.evac(xt[:, q4 * 4:(q4 + 1) * 4, :], pb[:].rearrange("q (k t) -> q k t", k=4), [pk], [xtk])
        pk, pb = p.bank()
        for kc in range(KC):
            p.mm(pb[:, 0:16], xt[:, kc, :], wr[:, kc, :], kc == 0, kc == KC - 1, [xtk, 'wr'], [pk])
        p.I('dve', 'tensor_tensor', [pk, 'rbb'], ['lg'], out=lg[:, tb, :], in0=pb[:, 0:16], in1=rbb[:], op=ALU.add)
    emit_routing(p, c, cd, idx_tab, RT)


def alloc_routing(p):
    RT = {}
    nb = NT // 128
    for nm, shp, dt in [('lg', [128, nb, 16], F32), ('ex', [128, nb, 16], F32), ('t16a', [128, nb, 16], F32),
                        ('t16b', [128, nb, 16], F32), ('sel', [128, nb, 16], F32), ('gate', [128, nb, 16], F32),
                        ('pos', [128, nb, 16], F32), ('m', [128, nb], F32), ('m1', [128, nb * 4], F32),
                        ('m2', [128, nb * 4], F32), ('sc', [128, nb * 4], F32), ('gs', [128, nb * 4], F32),
                        ('selb', [128, nb, 16], BF16), ('carry', [128, 16], F32), ('idxf', [128, nb, 16], F32),
                        ('eA', [128, nb], F32), ('eB', [128, nb], F32), ('posA', [128, nb], F32),
                        ('posB', [128, nb], F32), ('slotA', [128, nb], F32), ('slotB', [128, nb], F32),
                        ('gA', [128, nb], F32), ('gB', [128, nb], F32), ('okA', [128, nb], F32),
                        ('okB', [128, nb], F32), ('slotAi', [128, nb], I32), ('slotBi', [128, nb], I32),
                        ('tokid', [128, nb], I32), ('fill', [128, NSLOT // 128], I32),
                        ('ustr', [128, 128], BF16), ('onesb', [128, 128], BF16), ('ustrf', [128, 128], F32)]:
        RT[nm] = p.sb(shp, dt, 'rt_' + nm)
    return RT


def emit_routing(p, c, cd, idx_tab, RT):
    nb = NT // 128
    R = RT
    K = 'rt'

    def dv(method, **kw):
        p.I('dve', method, [K, 'lg'], [K], **kw)

    p.dma('sp', R['idxf'][:], cd['idxf'], writes=[K])
    p.dma('sp', R['tokid'][:], cd['tokid'], writes=[K])
    p.dma('sp', R['ustrf'][:], cd['ustrict'], writes=[K])
    p.dma('sp', R['fill'][:], cd['fill'], writes=[K])
    p.dma('sp', idx_tab.rearrange("(q j) o -> q (j o)", q=128), R['fill'][:], reads=[K], writes=['idx_init'])
    dv('tensor_copy', out=R['ustr'][:], in_=R['ustrf'][:])
    dv('memset', ap=R['onesb'][:], constant=1.0)
    lg, ex = R['lg'], R['ex']
    dv('tensor_reduce', out=R['m'][:], in_=lg[:], axis=AX.X, op=ALU.max)
    dv('tensor_tensor', out=ex[:], in0=lg[:], in1=R['m'][:].unsqueeze(2).to_broadcast([128, nb, 16]), op=ALU.subtract)
    p.I('act', 'activation', [K], [K], out=ex[:], in_=ex[:], func=AF.Exp)
    ex3 = ex[:].rearrange("q b (g i) -> q (b g) i", g=4)
    a3 = R['t16a'][:].rearrange("q b (g i) -> q (b g) i", g=4)
    b3 = R['t16b'][:].rearrange("q b (g i) -> q (b g) i", g=4)
    bc4 = lambda t: t[:].unsqueeze(2).to_broadcast([128, nb * 4, 4])
    dv('tensor_reduce', out=R['m1'][:], in_=ex3, axis=AX.X, op=ALU.max)
    dv('tensor_tensor', out=a3, in0=ex3, in1=bc4(R['m1']), op=ALU.is_equal)
    dv('scalar_tensor_tensor', out=b3, in0=a3, scalar=-1.0e30, in1=ex3, op0=ALU.mult, op1=ALU.add)
    dv('tensor_reduce', out=R['m2'][:], in_=b3, axis=AX.X, op=ALU.max)
    dv('tensor_tensor', out=R['sc'][:], in0=R['m1'][:], in1=R['m2'][:], op=ALU.add)
    sc3 = R['sc'][:].rearrange("q (b g) -> q b g", g=4)
    dv('tensor_reduce', out=R['m'][:], in_=sc3, axis=AX.X, op=ALU.max)
    dv('tensor_tensor', out=R['gs'][:].rearrange("q (b g) -> q b g", g=4), in0=sc3,
       in1=R['m'][:].unsqueeze(2).to_broadcast([128, nb, 4]), op=ALU.is_equal)
    dv('tensor_tensor', out=a3, in0=ex3, in1=bc4(R['m2']), op=ALU.is_ge)
    sel3 = R['sel'][:].rearrange("q b (g i) -> q (b g) i", g=4)
    dv('tensor_tensor', out=sel3, in0=a3, in1=bc4(R['gs']), op=ALU.mult)
    dv('tensor_copy', out=R['selb'][:], in_=R['sel'][:])
    dv('tensor_tensor', out=R['t16a'][:], in0=R['sel'][:], in1=ex[:], op=ALU.mult)
    dv('tensor_reduce', out=R['m'][:], in_=R['t16a'][:], axis=AX.X, op=ALU.add)
    dv('reciprocal', out=R['m'][:], in_=R['m'][:])
    dv('tensor_tensor', out=R['gate'][:], in0=R['t16a'][:],
       in1=R['m'][:].unsqueeze(2).to_broadcast([128, nb, 16]), op=ALU.mult)
    dv('memset', ap=R['carry'][:], constant=0.0)
    for b in range(nb):
        pk, pb = p.bank()
        p.mm(pb[:, 0:16], R['ustr'][:], R['selb'][:, b, :], True, True, [K], [pk])
        p.mm(pb[:, 16:32], R['onesb'][:], R['selb'][:, b, :], True, True, [K], [pk])
        p.I('dve', 'tensor_tensor', [pk, K], [K], out=R['pos'][:, b, :], in0=pb[:, 0:16], in1=R['carry'][:], op=ALU.add)
        p.I('dve', 'tensor_tensor', [pk, K], [K], out=R['carry'][:], in0=pb[:, 16:32], in1=R['carry'][:], op=ALU.add)
    idxf = R['idxf']
    bc16 = lambda t: t[:].unsqueeze(2).to_broadcast([128, nb, 16])
    dv('tensor_tensor', out=R['t16a'][:], in0=R['sel'][:], in1=idxf[:], op=ALU.mult)
    dv('tensor_reduce', out=R['eB'][:], in_=R['t16a'][:], axis=AX.X, op=ALU.max)
    dv('tensor_scalar', out=R['t16b'][:], in0=idxf[:], scalar1=-1.0, scalar2=15.0, op0=ALU.mult, op1=ALU.add)
    dv('tensor_tensor', out=R['t16b'][:], in0=R['t16b'][:], in1=R['sel'][:], op=ALU.mult)
    dv('tensor_reduce', out=R['eA'][:], in_=R['t16b'][:], axis=AX.X, op=ALU.max)
    dv('tensor_scalar', out=R['eA'][:], in0=R['eA'][:], scalar1=-1.0, scalar2=15.0, op0=ALU.mult, op1=ALU.add)
    dv('scalar_tensor_tensor', out=R['t16b'][:], in0=idxf[:], scalar=float(CAP), in1=R['pos'][:], op0=ALU.mult, op1=ALU.add)
    for s in ('A', 'B'):
        dv('tensor_tensor', out=R['t16a'][:], in0=idxf[:], in1=bc16(R['e' + s]), op=ALU.is_equal)
        dv('tensor_tensor', out=R['ex'][:], in0=R['t16a'][:], in1=R['pos'][:], op=ALU.mult)
        dv('tensor_reduce', out=R['pos' + s][:], in_=R['ex'][:], axis=AX.X, op=ALU.add)
        dv('tensor_tensor', out=R['ex'][:], in0=R['t16a'][:], in1=R['t16b'][:], op=ALU.mult)
        dv('tensor_reduce', out=R['slot' + s][:], in_=R['ex'][:], axis=AX.X, op=ALU.add)
        dv('tensor_tensor', out=R['ex'][:], in0=R['t16a'][:], in1=R['gate'][:], op=ALU.mult)
        dv('tensor_reduce', out=R['g' + s][:], in_=R['ex'][:], axis=AX.X, op=ALU.add)
        dv('tensor_scalar', out=R['ok' + s][:], in0=R['pos' + s][:], scalar1=float(CAP), scalar2=None, op0=ALU.is_lt)
        dv('tensor_tensor', out=R['g' + s][:], in0=R['g' + s][:], in1=R['ok' + s][:], op=ALU.mult)
        dv('tensor_scalar', out=R['ok' + s][:], in0=R['ok' + s][:], scalar1=-BIGSLOT, scalar2=BIGSLOT, op0=ALU.mult, op1=ALU.add)
        dv('tensor_tensor', out=R['slot' + s][:], in0=R['slot' + s][:], in1=R['ok' + s][:], op=ALU.add)
        dv('tensor_copy', out=R['slot' + s + 'i'][:], in_=R['slot' + s][:])
    n = 0
    for b in range(nb):
        for s in ('A', 'B'):
            p.scatter(idx_tab, R['slot' + s + 'i'][:, b:b + 1], R['tokid'][:, b:b + 1], NSLOT - 1,
                      [K, 'idx_init'], [('idx_w', n)], key=('idx_sc', n % 4))
            n += 1
    RT['idx_keys'] = [('idx_w', i) for i in range(n)] + ['idx_init']


def stage_moe(p, c, xm_bf, idx_tab, RT, wg, wu, wd, yslots):
    nsb = CAP // 128
    nch = [(0, 512), (512, CAP)] if CAP > 512 else [(0, CAP)]
    xg = [p.sb([128, D], BF16, f'me_xg{i}') for i in range(2)]
    idxs = [p.sb([128, 1], I32, f'me_ix{i}') for i in range(4)]
    xTe = [p.sb([128, KC, CAP], BF16, f'me_xT{i}') for i in range(2)]
    hT = p.sb([128, KC, CAP], BF16, 'me_hT')
    wts = [p.sb([128, KC, 512], BF16, f'me_w{i}') for i in range(5)]
    sil = [p.sb([128, CAP], BF16, f'me_sil{i}') for i in range(2)]
    yst = [p.sb([128, 512], F32, f'me_y{i}') for i in range(4)]
    for i in range(2):
        p.I('dve', 'memset', [], [('me_xg', i)], ap=xg[i][:], constant=0.0)
    wi = 0
    gi = 0
    yi = 0

    def wload(src):
        nonlocal wi
        w = wts[wi % 5]
        wk = ('me_w', wi % 5)
        wi += 1
        p.dma('pool', w[:], src.rearrange("(kc q) f -> q kc f", q=128), writes=[wk], key=('me_wld', wk))
        return w, wk

    for e in range(16):
        xT = xTe[e % 2]
        xTk = ('me_xT', e % 2)
        for sb in range(nsb):
            ix = idxs[gi % 4]
            ixk = ('me_ix', gi % 4)
            g = xg[gi % 2]
            gk = ('me_xg', gi % 2)
            gi += 1
            r0 = e * CAP + sb * 128
            p.dma('sp', ix[:], idx_tab[r0:r0 + 128, :], reads=RT['idx_keys'], writes=[ixk])
            p.gather(g[:], xm_bf, ix[:, 0:1], NT, [ixk, 'xm_bf'], [gk])
            for half in range(2):
                pk, pb = p.bank()
                pbv = pb[:].bitcast(BF16)
                for j in range(8):
                    kc = half * 8 + j
                    p.tr(pbv[:, j * 128:(j + 1) * 128], g[:, kc * 128:(kc + 1) * 128], c.identb[:], [gk, 'identb'], [pk])
                p.evac(xT[:, half * 8:(half + 1) * 8, sb * 128:(sb + 1) * 128],
                       pbv.rearrange("q (k t) -> q k t", k=8), [pk], [xTk])
        for ft in range(4):
            wgt, wgk = wload(wg[e, :, ft * 512:(ft + 1) * 512])
            wut, wuk = wload(wu[e, :, ft * 512:(ft + 1) * 512])
            for fb in range(4):
                fc = ft * 4 + fb
                gb = [p.bank() for _ in nch]
                ub = [p.bank() for _ in nch]
                for ci, (c0, c1) in enumerate(nch):
                    for kc in range(KC):
                        p.mm(gb[ci][1][:, 0:c1 - c0], wgt[:, kc, fb * 128:(fb + 1) * 128], xT[:, kc, c0:c1],
                             kc == 0, kc == KC - 1, [wgk, xTk], [gb[ci][0]])
                for ci, (c0, c1) in enumerate(nch):
                    for kc in range(KC):
                        p.mm(ub[ci][1][:, 0:c1 - c0], wut[:, kc, fb * 128:(fb + 1) * 128], xT[:, kc, c0:c1],
                             kc == 0, kc == KC - 1, [wuk, xTk], [ub[ci][0]])
                s = sil[fc % 2]
                sk = ('me_sil', fc % 2)
                for ci, (c0, c1) in enumerate(nch):
                    p.I('act', 'activation', [gb[ci][0]], [sk], out=s[:, c0:c1], in_=gb[ci][1][:, 0:c1 - c0], func=AF.Silu)
                for ci, (c0, c1) in enumerate(nch):
                    p.I('dve', 'tensor_tensor', [ub[ci][0], sk], ['me_hT'], out=hT[:, fc, c0:c1], in0=ub[ci][1][:, 0:c1 - c0],
                        in1=s[:, c0:c1], op=ALU.mult)
        for ct in range(4):
            wdt, wdk = wload(wd[e, :, ct * 512:(ct + 1) * 512])
            for sb in range(nsb):
                pk, pb = p.bank()
                for fc in range(KC):
                    p.mm(pb[:], hT[:, fc, sb * 128:(sb + 1) * 128], wdt[:, fc, :], fc == 0, fc == KC - 1,
                         ['me_hT', wdk], [pk])
                y = yst[yi % 4]
                yk = ('me_y', yi % 4)
                yi += 1
                p.evac(y[:], pb[:], [pk], [yk])
                r0 = e * CAP + sb * 128
                p.dma('sp', yslots[r0:r0 + 128, ct * 512:(ct + 1) * 512], y[:], reads=[yk], writes=['yslots'])


def stage_tail3(p, c, xm_f32, yslots, RT, lng, lnb, out_rows, out_key, xT_out=None):
    G = p.sb([128, D], F32, 't3_G')
    B = p.sb([128, D], F32, 't3_B')
    p.dma('sp', G[:], lng.partition_broadcast(128), writes=['lnG'])
    p.dma('sp', B[:], lnb.partition_broadcast(128), writes=['lnB'])
    ya = [p.sb([128, D], F32, f't3_ya{i}') for i in range(2)]
    yb = [p.sb([128, D], F32, f't3_yb{i}') for i in range(2)]
    zs = [p.sb([128, D], F32, f't3_z{i}') for i in range(2)]
    for i in range(2):
        p.I('dve', 'memset', [], [('t3_ya', i)], ap=ya[i][:], constant=0.0)
        p.I('dve', 'memset', [], [('t3_yb', i)], ap=yb[i][:], constant=0.0)
    scr = ln_scratch(p, 't3')
    if xT_out is not None:
        zb = [p.sb([128, D], BF16, f't3_zb{i}') for i in range(2)]
        stg = [p.sb([128, KC, 512], BF16, f't3_stg{i}') for i in range(2)]
        xT_v = xT_out.rearrange("(kc q) t -> q kc t", q=128)
    nblk = NT // 128
    for tb in range(nblk):
        a, ak = ya[tb % 2], ('t3_ya', tb % 2)
        b, bk = yb[tb % 2], ('t3_yb', tb % 2)
        z, zk = zs[tb % 2], ('t3_z', tb % 2)
        p.gather(a[:], yslots, RT['slotAi'][:, tb:tb + 1], NSLOT - 1, ['rt', 'yslots'], [ak])
        p.gather(b[:], yslots, RT['slotBi'][:, tb:tb + 1], NSLOT - 1, ['rt', 'yslots'], [bk])
        p.dma('sp', z[:], xm_f32[tb * 128:(tb + 1) * 128, :], reads=['xm_f32'], writes=[zk])
        p.I('act', 'mul', [zk], [zk], out=z[:], in_=z[:], mul=ALPHA)
        p.I('dve', 'scalar_tensor_tensor', [zk, ak, 'rt'], [zk], out=z[:], in0=a[:], scalar=RT['gA'][:, tb:tb + 1],
            in1=z[:], op0=ALU.mult, op1=ALU.add)
        p.I('dve', 'scalar_tensor_tensor', [zk, bk, 'rt'], [zk], out=z[:], in0=b[:], scalar=RT['gB'][:, tb:tb + 1],
            in1=z[:], op0=ALU.mult, op1=ALU.add)
        emit_ln(p, z, zk, G, B, 't3', scr)
        p.dma('sp', out_rows[tb * 128:(tb + 1) * 128, :], z[:], reads=[zk], writes=[out_key])
        if xT_out is not None:
            zbb, zbk = zb[tb % 2], ('t3_zb', tb % 2)
            p.I('act', 'copy', [zk], [zbk], out=zbb[:], in_=z[:])
            tile_i = tb // 4
            s, sk = stg[tile_i % 2], ('t3_stg', tile_i % 2)
            for half in range(2):
                pk, pb = p.bank()
                pbv = pb[:].bitcast(BF16)
                for j in range(8):
                    kc = half * 8 + j
                    p.tr(pbv[:, j * 128:(j + 1) * 128], zbb[:, kc * 128:(kc + 1) * 128], c.identb[:], [zbk, 'identb'], [pk])
                p.evac(s[:, half * 8:(half + 1) * 8, (tb % 4) * 128:(tb % 4 + 1) * 128],
                       pbv.rearrange("q (k t) -> q k t", k=8), [pk], [sk])
            if tb % 4 == 3:
                p.dma('sp', xT_v[:, :, tile_i * 512:(tile_i + 1) * 512], s[:], reads=[sk], writes=['x1T'])


def const_inputs(nc):
    din = lambda n, s, d=F32: nc.dram_tensor(n, s, d, kind="ExternalInput").ap()
    nb = NT // 128
    return dict(ident=din('c_ident', [128, 128]), ustrict=din('c_ustrict', [128, 128]),
                idxf=din('c_idxf', [128, nb, 16]), tokid=din('c_tokid', [128, nb], I32),
                fill=din('c_fill', [128, NSLOT // 128], I32))


def const_arrays():
    nb = NT // 128
    us = np.triu(np.ones((128, 128), np.float32), 1)
    return dict(c_ident=np.eye(128, dtype=np.float32), c_ustrict=us,
                c_idxf=np.broadcast_to(np.arange(16, dtype=np.float32), (128, nb, 16)).copy(),
                c_tokid=(np.arange(nb, dtype=np.int32)[None, :] * 128 + np.arange(128, dtype=np.int32)[:, None]).astype(np.int32),
                c_fill=np.full((128, NSLOT // 128), NT, np.int32))


def build_A(stages=('tr', 'proj', 'attn', 't1', 'moe', 't3'), debug=False):
    nc = bass.Bass("TRN2", target_bir_lowering=False)
    din = lambda n, s, d=F32: nc.dram_tensor(n, s, d, kind="ExternalInput").ap()
    dout = lambda n, s, d=F32: nc.dram_tensor(n, s, d, kind="ExternalOutput").ap()
    dint = (lambda n, s, d=F32: nc.dram_tensor(n, s, d, kind="Internal").ap()) if not debug else dout
    xin = din('xin', [NTH, D])
    wqkv = din('wqkv', [D, 3 * D])
    w_o = din('w_o', [D, D])
    biasT = din('biasT', [128, 32, 256])
    cvec = din('cvec', [32])
    kvalid = din('kvalid', [128, NTH // 128])
    lng = din('lng', [2, D])
    lnb = din('lnb', [2, D])
    rw = din('rw', [D, 16])
    rb = din('rb', [16])
    wg = din('wg', [16, D, D])
    wu = din('wu', [16, D, D])
    wd = din('wd', [16, D, D])
    cd = const_inputs(nc)
    x1 = dout('x1', [NT, D])
    x1T = dout('x1T', [D, NT], BF16)
    xT = dint('xT', [D, NTH], BF16)
    QT = dint('QT', [D, NT], BF16)
    KT = dint('KT', [D, NTH], BF16)
    V = dint('V', [NTH, D], BF16)
    aoT = dint('aoT', [D, NT], BF16)
    xm_f32 = dint('xm_f32', [NT, D])
    xm_bf = dint('xm_bf', [NT + 1, D], BF16)
    idx_tab = dint('idx_tab', [NSLOT, 1], I32)
    yslots = dint('yslots', [NSLOT, D])
    p = Prog(nc)
    p.init_arena(206 * 1024)
    p.make_banks()
    c = load_consts(p, cd)
    RT = alloc_routing(p)
    if debug:
        dbg_rt = dout('dbg_rt', [128, 8, NT // 128])
    m0 = p.mark()
    if 'tr' in stages:
        stage_transpose(p, c, xin, NTH, xT, 'xT', 'tr')
        p.release(m0)
    if 'proj' in stages:
        wres = p.sb([128, KC, D], BF16, 'pj_w')
        bufs = ([p.sb([128, KC, 512], BF16, f'pj_x{i}') for i in range(2)],
                [p.sb([128, KC, 512], BF16, f'pj_o{i}') for i in range(2)])
        bufs_tm = (bufs[0], [p.sb([128, D], BF16, f'pj_ot{i}') for i in range(2)])
        load_wres(p, wres, 'pj_w', wqkv[:, 0:D], D)
        stage_proj(p, xT[:, HALO:], 'xT', NT, wres, 'pj_w', D, 'fm', QT, 'QT', 'pq', bufs)
        load_wres(p, wres, 'pj_w', wqkv[:, D:2 * D], D)
        stage_proj(p, xT, 'xT', NTH, wres, 'pj_w', D, 'fm', KT, 'KT', 'pk', bufs)
        load_wres(p, wres, 'pj_w', wqkv[:, 2 * D:3 * D], D)
        stage_proj(p, xT, 'xT', NTH, wres, 'pj_w', D, 'tm', V, 'V', 'pv', bufs_tm)
        p.release(m0)
    if 'attn' in stages:
        stage_attnA(p, c, QT, KT, V, kvalid, biasT, cvec, aoT)
        p.release(m0)
    if 't1' in stages:
        zrow = p.sb([1, D], BF16, 'zrow')
        p.I('dve', 'memset', [], ['zrow'], ap=zrow[:], constant=0.0)
        p.dma('sp', xm_bf[NT:NT + 1, :], zrow[:], reads=['zrow'], writes=['xm_bf'])
        stage_tail1(p, c, aoT, 'aoT', xin[HALO:, :], w_o, lng[0], lnb[0], rw, rb, cd, xm_f32, xm_bf, idx_tab, RT)
        if debug:
            for i, nm in enumerate(['eA', 'eB', 'gA', 'gB', 'posA', 'posB', 'slotA', 'slotB']):
                p.dma('sp', dbg_rt[:, i, :], RT[nm][:], reads=['rt'], writes=['dbg_rt'])
        p.release(m0)
    if 'moe' in stages:
        stage_moe(p, c, xm_bf, idx_tab, RT, wg, wu, wd, yslots)
        p.release(m0)
    if 't3' in stages:
        stage_tail3(p, c, xm_f32, yslots, RT, lng[1], lnb[1], x1, 'x1', xT_out=x1T)
    p.barrier()
    p.emit()
    return nc, p


def prep_A(inputs, core):
    b, j = divmod(core, 4)
    x = inputs['x']
    t0 = j * NT
    xin = np.zeros((NTH, D), np.float32)
    if j > 0:
        xin[:] = x[b, t0 - HALO:t0 + NT]
    else:
        xin[HALO:] = x[b, 0:NT]
    kv = np.ones((NTH,), np.float32)
    if j == 0:
        kv[:HALO] = 0.0
    kvalid = np.ascontiguousarray(kv.reshape(NTH // 128, 128).T)
    rbias = inputs['a_rel_bias'][0]
    s = np.arange(128)[:, None]
    t = np.arange(128)[None, :]
    i3 = np.minimum(t - s + 128, 128) + 128
    i4 = t - s + 128
    biasT = np.concatenate([rbias[:, i3], rbias[:, i4]], axis=2)
    biasT = np.ascontiguousarray(biasT.transpose(1, 0, 2))
    d = dict(xin=xin, wqkv=inputs['a_w_qkv'][0], w_o=inputs['a_w_o'][0], biasT=biasT,
             cvec=np.ascontiguousarray(rbias[:, 256]), kvalid=kvalid,
             lng=inputs['ln_gain'][0], lnb=inputs['ln_bias'][0], rw=inputs['router_w'], rb=inputs['router_b'],
             wg=inputs['moe_w_gate'][0], wu=inputs['moe_w_up'][0], wd=inputs['moe_w_down'][0])
    d.update(const_arrays())
    return d


NTOK = 2 * SEQ
QBLK = 512
B1_DBG = 9


def build_B1(nunits=4, nqt=SEQ // QBLK):
    nc = bass.Bass("TRN2", target_bir_lowering=False)
    din = lambda n, s, d=F32: nc.dram_tensor(n, s, d, kind="ExternalInput").ap()
    dout = lambda n, s, d=F32: nc.dram_tensor(n, s, d, kind="ExternalOutput").ap()
    x1T = din('x1T', [D, NTOK], BF16)
    wq2 = din('wq2', [D, 256])
    wk2 = din('wk2', [D, 256])
    wv2 = din('wv2', [D, 256])
    wf2 = din('wf2', [D, 16])
    fgb = din('fgb', [2])
    tri_d = din('c_tri', [128, 128])
    trim_d = din('c_trimask', [128, 128])
    aoT2 = dout('aoT2', [256, NTOK], BF16)
    p = Prog(nc)
    p.init_arena(206 * 1024)
    p.make_banks()
    wq = p.sb([128, KC, 256], BF16, 'b1_wq')
    wk = p.sb([128, KC, 256], BF16, 'b1_wk')
    wv = p.sb([128, KC, 256], BF16, 'b1_wv')
    wf = p.sb([128, KC, 16], BF16, 'b1_wf')
    for w, src, nm in ((wq, wq2, 'b1_wq'), (wk, wk2, 'b1_wk'), (wv, wv2, 'b1_wv'), (wf, wf2, 'b1_wf')):
        p.dma('pool', w[:], src.rearrange("(kc q) f -> q kc f", q=128), writes=[nm])
    trif = p.sb([128, 128], F32, 'b1_tri')
    onesf = p.sb([128, 128], F32, 'b1_onesf')
    onesb = p.sb([128, 128], BF16, 'b1_onesb')
    trimf = p.sb([128, 128], F32, 'b1_trimf')
    trimb = p.sb([128, 128], BF16, 'b1_trimb')
    nfgb = p.sb([128, 2], F32, 'b1_nfgb')
    p.dma('sp', trif[:], tri_d, writes=['tri'])
    p.dma('sp', trimf[:], trim_d, writes=['trimf'])
    p.dma('sp', nfgb[:], fgb.partition_broadcast(128), writes=['nfgb'])
    p.I('dve', 'tensor_scalar', ['nfgb'], ['nfgb'], out=nfgb[:], in0=nfgb[:], scalar1=-1.0, scalar2=None, op0=ALU.mult)
    p.I('dve', 'memset', [], ['onesf'], ap=onesf[:], constant=1.0)
    p.I('dve', 'memset', [], ['onesb'], ap=onesb[:], constant=1.0)
    p.I('dve', 'tensor_copy', ['trimf'], ['trimb'], out=trimb[:], in_=trimf[:])
    QTu = p.sb([128, SEQ], BF16, 'b1_Q')
    KTu = p.sb([128, SEQ], BF16, 'b1_K')
    Vu = p.sb([128, SEQ // 128, 128], BF16, 'b1_V')
    xts = [p.sb([128, KC, 512], BF16, f'b1_x{i}') for i in range(2)]
    nblk = SEQ // 128
    lfz = p.sb([128, nblk], F32, 'b1_lfz')
    lf = p.sb([128, nblk], F32, 'b1_lf')
    fin = p.sb([128, nblk], F32, 'b1_fin')
    tot = p.sb([128, nblk], F32, 'b1_tot')
    sc = [p.sb([128, nblk], F32, f'b1_sc{i}') for i in range(2)]
    offi = p.sb([128, nblk], F32, 'b1_offi')
    fneg = p.sb([128, nblk], F32, 'b1_fneg')
    nbs = [p.sb([128, nblk], F32, f'b1_nb{i}') for i in range(2)]
    pts = [p.sb([128, 512], BF16, f'b1_p{i}') for i in range(4)]
    rds = [p.sb([128, 512], F32, f'b1_rd{i}') for i in range(2)]
    osts = [p.sb([128, 512], BF16, f'b1_o{i}') for i in range(2)]
    x1T_v = x1T.rearrange("(kc q) t -> q kc t", q=128)
    scale = 1.0 / math.sqrt(128.0)
    sbank = 0
    pi = 0
    for u in range(nunits):
        b, hh = divmod(u, 2)
        for tt in range(SEQ // 512):
            xt = xts[tt % 2]
            xk = ('b1_x', tt % 2)
            g0 = b * SEQ + tt * 512
            p.dma('sp', xt[:], x1T_v[:, :, g0:g0 + 512], writes=[xk])
            for (wres, wkey, dst, dk) in ((wq, 'b1_wq', QTu, 'Q'), (wk, 'b1_wk', KTu, 'K')):
                pk, pb = p.bank()
                for kc in range(KC):
                    p.mm(pb[:], wres[:, kc, hh * 128:(hh + 1) * 128], xt[:, kc, :], kc == 0, kc == KC - 1, [wkey, xk], [pk])
                p.evac(dst[:, tt * 512:(tt + 1) * 512], pb[:], [pk], [dk])
            for tb in range(4 if B1_DBG >= 2 else 0):
                blk = tt * 4 + tb
                pk, pb = p.bank()
                for kc in range(KC):
                    p.mm(pb[:, 0:128], xt[:, kc, tb * 128:(tb + 1) * 128], wv[:, kc, hh * 128:(hh + 1) * 128],
                         kc == 0, kc == KC - 1, ['b1_wv', xk], [pk])
                for kc in range(KC if B1_DBG != 21 else 0):
                    p.mm(pb[:, 128:144], xt[:, kc, tb * 128:(tb + 1) * 128], wf[:, kc, :],
                         kc == 0, kc == KC - 1, ['b1_wf', xk], [pk])
                ev = ('act', 'dve')[blk % 2]
                p.evac(Vu[:, blk, :], pb[:, 0:128], [pk], ['V'], eng=ev)
                p.evac(lfz[:, blk:blk + 1], pb[:, 128 + hh:129 + hh], [pk], ['lfz'], eng=ev)
        if B1_DBG < 3:
            continue
        p.I('act', 'activation', ['lfz', 'nfgb'], ['lf'], out=lf[:], in_=lfz[:], func=AF.Exp, scale=-1.0, bias=nfgb[:, hh:hh + 1])
        p.I('act', 'activation', ['lf'], ['lf'], out=lf[:], in_=lf[:], func=AF.Ln, bias=1.0, scale=1.0)
        p.I('dve', 'tensor_scalar', ['lf'], ['lf'], out=lf[:], in0=lf[:], scalar1=-1.0, scalar2=None, op0=ALU.mult)
        pk, pb = p.bank()
        p.mm(pb[:, 0:nblk], trif[:], lf[:], True, True, ['tri', 'lf'], [pk])
        p.mm(pb[:, nblk:2 * nblk], onesf[:], lf[:], True, True, ['onesf', 'lf'], [pk])
        p.I('dve', 'tensor_copy', [pk], ['fin'], out=fin[:], in_=pb[:, 0:nblk])
        p.I('dve', 'tensor_copy', [pk], ['tot'], out=tot[:], in_=pb[:, nblk:2 * nblk])
        p.I('dve', 'tensor_copy', ['tot'], ['sc0'], out=sc[0][:], in_=tot[:])
        k = 1
        cur = 0
        while k < nblk:
            a, bb = sc[cur], sc[1 - cur]
            ak, bk = f'sc{cur}', f'sc{1 - cur}'
            p.I('dve', 'tensor_copy', [ak], [bk], out=bb[:, 0:k], in_=a[:, 0:k])
            p.I('dve', 'tensor_tensor', [ak], [bk], out=bb[:, k:nblk], in0=a[:, k:nblk], in1=a[:, 0:nblk - k], op=ALU.add)
            cur = 1 - cur
            k *= 2
        p.I('dve', 'tensor_copy', [f'sc{cur}'], ['offi'], out=offi[:], in_=sc[cur][:])
        p.I('dve', 'tensor_tensor', ['fin', 'offi'], ['fneg'], out=fneg[:], in0=fin[:], in1=offi[:], op=ALU.add)
        p.I('dve', 'tensor_tensor', ['fneg', 'tot'], ['fneg'], out=fneg[:], in0=tot[:], in1=fneg[:], op=ALU.subtract)
        tiles = []
        for qt in range(nqt):
            nk = 4 * qt + 4
            for kb in range(nk):
                tiles.append((qt, kb, nk))
        stt = {}

        def qk_part(tile):
            nonlocal sbank, pi
            qt, kb, nk = tile
            if kb == 0:
                nb = nbs[qt % 2]
                nbk = ('b1_nb', qt % 2)
                r = 4 * qt + 1
                p.I('dve', 'tensor_scalar', ['fneg', 'offi'], [nbk], out=nb[:, 0:nk], in0=fneg[:, 0:nk],
                    scalar1=offi[:, r:r + 1], scalar2=None, op0=ALU.add)
            i = kb - 4 * qt
            c0 = i * 128 if i > 0 else 0
            q0 = qt * 512
            sk_, sb_ = ('ps', sbank % 4), p.banks[sbank % 4]
            sbank += 1
            p.mm(sb_[:, c0:512], KTu[:, kb * 128:(kb + 1) * 128], QTu[:, q0 + c0:q0 + 512], True, True, ['K', 'Q'], [sk_])
            stt[tile] = (sk_, sb_, c0, i)

        def rest_part(tile):
            nonlocal pi
            qt, kb, nk = tile
            sk_, sb_, c0, i = stt.pop(tile)
            nb = nbs[qt % 2]
            nbk = ('b1_nb', qt % 2)
            ok_, ob = ('ps', 4 + qt % 2), p.banks[4 + qt % 2]
            lk_, lb = ('ps', 6 + qt % 2), p.banks[6 + qt % 2]
            pt = pts[pi % 4]
            ptk = ('b1_p', pi % 4)
            pi += 1
            p.I('act', 'activation', [sk_, nbk], [ptk], out=pt[:, c0:512], in_=sb_[:, c0:512], func=AF.Exp,
                scale=scale, bias=nb[:, kb:kb + 1])
            if i >= 0:
                p.I('dve', 'tensor_tensor', [ptk, 'trimb'], [ptk], out=pt[:, c0:c0 + 128], in0=pt[:, c0:c0 + 128],
                    in1=trimb[:], op=ALU.mult)
            p.mm(ob[:, c0:512], Vu[:, kb, :], pt[:, c0:512], kb == 0, kb == nk - 1, ['V', ptk], [ok_])
            p.mm(lb[:, c0:512], onesb[:], pt[:, c0:512], kb == 0, kb == nk - 1, ['onesb', ptk], [lk_])
            if kb == nk - 1:
                rd = rds[qt % 2]
                rk = ('b1_rd', qt % 2)
                p.I('dve', 'reciprocal', [lk_], [rk], out=rd[:], in_=lb[:])
                ost = osts[qt % 2]
                osk = ('b1_o', qt % 2)
                p.I('dve', 'tensor_tensor', [ok_, rk], [osk], out=ost[:], in0=ob[:], in1=rd[:], op=ALU.mult)
                g0 = b * SEQ + qt * 512
                p.dma('sp', aoT2[hh * 128:(hh + 1) * 128, g0:g0 + 512], ost[:], reads=[osk], writes=['aoT2'])

        LA = 2
        for ii in range(len(tiles) + LA):
            if ii < len(tiles):
                qk_part(tiles[ii])
            if ii >= LA:
                rest_part(tiles[ii - LA])
    p.barrier()
    p.emit()
    return nc, p


def prep_B1(inputs, core, x1T_full):
    c = core
    kvw = inputs['kv_w']
    wv = kvw[:, D + 256 * c:D + 256 * c + 256]
    wf = np.zeros((D, 16), np.float32)
    wf[:, 0:2] = inputs['fg_w'][:, 2 * c:2 * c + 2]
    tri = np.triu(np.ones((128, 128), np.float32), 0)
    return dict(x1T=x1T_full, wq2=np.ascontiguousarray(inputs['b_w_q'][0][:, 256 * c:256 * c + 256]),
                wk2=np.ascontiguousarray(kvw[:, 256 * c:256 * c + 256]), wv2=np.ascontiguousarray(wv), wf2=wf,
                fgb=np.ascontiguousarray(inputs['fg_b'][2 * c:2 * c + 2]), c_tri=tri, c_trimask=tri.copy())


def build_B2():
    nc = bass.Bass("TRN2", target_bir_lowering=False)
    din = lambda n, s, d=F32: nc.dram_tensor(n, s, d, kind="ExternalInput").ap()
    dout = lambda n, s, d=F32: nc.dram_tensor(n, s, d, kind="ExternalOutput").ap()
    dint = lambda n, s, d=F32: nc.dram_tensor(n, s, d, kind="Internal").ap()
    aoT = din('aoT', [D, NT], BF16)
    xres = din('xres', [NT, D])
    w_o = din('w_o', [D, D])
    lng = din('lng', [2, D])
    lnb = din('lnb', [2, D])
    rw = din('rw', [D, 16])
    rb = din('rb', [16])
    wg = din('wg', [16, D, D])
    wu = din('wu', [16, D, D])
    wd = din('wd', [16, D, D])
    cd = const_inputs(nc)
    out = dout('out', [NT, D])
    xm_f32 = dint('xm_f32', [NT, D])
    xm_bf = dint('xm_bf', [NT + 1, D], BF16)
    idx_tab = dint('idx_tab', [NSLOT, 1], I32)
    yslots = dint('yslots', [NSLOT, D])
    p = Prog(nc)
    p.init_arena(206 * 1024)
    p.make_banks()
    c = load_consts(p, cd)
    RT = alloc_routing(p)
    m0 = p.mark()
    zrow = p.sb([1, D], BF16, 'zrow')
    p.I('dve', 'memset', [], ['zrow'], ap=zrow[:], constant=0.0)
    p.dma('sp', xm_bf[NT:NT + 1, :], zrow[:], reads=['zrow'], writes=['xm_bf'])
    stage_tail1(p, c, aoT, 'aoT', xres, w_o, lng[0], lnb[0], rw, rb, cd, xm_f32, xm_bf, idx_tab, RT)
    p.release(m0)
    stage_moe(p, c, xm_bf, idx_tab, RT, wg, wu, wd, yslots)
    p.release(m0)
    stage_tail3(p, c, xm_f32, yslots, RT, lng[1], lnb[1], out, 'out')
    p.barrier()
    p.emit()
    return nc, p


def kernel(**inputs):
    inputs = {k: np.asarray(v) for k, v in inputs.items()}
    cores = list(range(NCORES))
    ncA, _ = build_A()
    resA = run_bass_kernel_spmd(ncA, [prep_A(inputs, c) for c in cores], core_ids=cores)
    x1 = [np.asarray(r['x1']) for r in resA.results]
    x1T_full = np.ascontiguousarray(np.concatenate([np.asarray(r['x1T']) for r in resA.results], axis=1))
    ncB1, _ = build_B1()
    resB1 = run_bass_kernel_spmd(ncB1, [prep_B1(inputs, c, x1T_full) for c in cores], core_ids=cores)
    aoT_full = np.concatenate([np.asarray(r['aoT2']) for r in resB1.results], axis=0)
    ncB2, _ = build_B2()
    cst = const_arrays()
    in2 = []
    for c in cores:
        d = dict(aoT=np.ascontiguousarray(aoT_full[:, c * NT:(c + 1) * NT]), xres=x1[c], w_o=inputs['b_w_o'][0],
                 lng=inputs['ln_gain'][1], lnb=inputs['ln_bias'][1], rw=inputs['router_w'], rb=inputs['router_b'],
                 wg=inputs['moe_w_gate'][1], wu=inputs['moe_w_up'][1], wd=inputs['moe_w_down'][1])
        d.update(cst)
        in2.append(d)
    resB2 = run_bass_kernel_spmd(ncB2, in2, core_ids=cores)
    out = np.stack([np.asarray(r['out']) for r in resB2.results], axis=0)
    return out.reshape(2, SEQ, D).astype(np.float32)
```
